# Optimizing a Trainium2 kernel written in Bass

```python
import jax, jax.numpy as jnp
from jax import lax
import numpy as np

D_MODEL = 1024
BATCH = 16
SEQ = 4096
DEPTH = 2

N_MEM = 256
D_MIX = D_MODEL
D_A = D_MIX // 4
D_B = D_MIX // 4
D_C = D_MIX // 2
H_A = 4
H_B = 4
H_C = 4
DH_A = D_A // H_A
DH_B = D_B // H_B
DH_C = D_C // H_C
CHUNK = 128
CONV_B = 31
CONV_C = 4
CONV_F = 3
N_XHEADS = 4
DH_X = D_MODEL // N_XHEADS
D_FF = ((8 * D_MODEL // 3 + 255) // 256) * 256
D_IN = 2 * D_A + 2 * D_B + 2 * D_C + 2 * H_C
RMS_EPS = 1e-6
LN_EPS = 1e-5

kernel_name = "hybrid_gmlp_conformer_mlstm_trunk"


def rmsnorm(x, g):
    xf = x.astype(jnp.float32)
    y = xf * lax.rsqrt(jnp.mean(xf * xf, axis=-1, keepdims=True) + RMS_EPS)
    return (y * g.astype(jnp.float32)).astype(x.dtype)


def standardize(x):
    xf = x.astype(jnp.float32)
    mu = jnp.mean(xf, axis=-1, keepdims=True)
    var = jnp.mean(jnp.square(xf - mu), axis=-1, keepdims=True)
    return (xf - mu) * lax.rsqrt(var + LN_EPS)


def causal_dwconv(x, w, b):
    K, C = w.shape
    y = lax.conv_general_dilated(
        x, w[:, None, :].astype(x.dtype), window_strides=(1,),
        padding=[(K - 1, 0)], dimension_numbers=('NWC', 'WIO', 'NWC'),
        feature_group_count=C)
    return y + b.astype(x.dtype)


def gmlp_spatial_gating(u, v, ln_g, ln_b, w_s, b_s):
    Bsz, S, _ = v.shape
    vn = (standardize(v) * ln_g + ln_b).astype(v.dtype)
    vn = vn.reshape(Bsz, S // CHUNK, CHUNK, H_A, DH_A)
    mask = jnp.tril(jnp.ones((CHUNK, CHUNK), dtype=bool))
    w = jnp.where(mask, w_s, 0.0).astype(v.dtype)
    s = jnp.einsum('hts,bcshd->bcthd', w, vn) + b_s.T.astype(v.dtype)[None, None, :, :, None]
    return u * s.reshape(Bsz, S, D_A)


def conformer_conv(a, g, conv_w, conv_b, gn_g, gn_b):
    Bsz, S, _ = a.shape
    y = a * jax.nn.sigmoid(g)
    y = causal_dwconv(y, conv_w, conv_b)
    yn = standardize(y.reshape(Bsz, S, H_B, DH_B)).reshape(Bsz, S, D_B)
    y = (yn * gn_g + gn_b).astype(a.dtype)
    return jax.nn.silu(y)


def mlstm_chunkwise(q, k, v, logi, logf):
    Bsz, H, S, DH = q.shape
    NC = S // CHUNK
    q = q * (DH ** -0.5)

    def to_chunks(a):
        a = a.reshape((Bsz, H, NC, CHUNK) + a.shape[3:])
        return jnp.moveaxis(a, 2, 0)

    qc, kc, vc, ic, fc = (to_chunks(t) for t in (q, k, v, logi, logf))
    mask = jnp.tril(jnp.ones((CHUNK, CHUNK), dtype=bool))

    def step(carry, inp):
        C, n, m = carry
        qt, kt, vt, it, ft = inp
        bcum = jnp.cumsum(ft, axis=-1)
        dmat = bcum[..., :, None] - bcum[..., None, :] + it[..., None, :]
        dmat = jnp.where(mask, dmat, -jnp.inf)
        inter = bcum + m[..., None]
        m_t = jnp.maximum(inter, jnp.max(dmat, axis=-1))
        w_intra = jnp.exp(dmat - m_t[..., None])
        w_inter = jnp.exp(inter - m_t)
        scores = jnp.einsum('bhtd,bhsd->bhts', qt, kt) * w_intra
        num = (w_inter[..., None] * jnp.einsum('bhtd,bhde->bhte', qt, C)
               + jnp.einsum('bhts,bhse->bhte', scores, vt))
        den = w_inter * jnp.einsum('bhtd,bhd->bht', qt, n) + jnp.sum(scores, axis=-1)
        h = num / jnp.maximum(jnp.abs(den), jnp.exp(-m_t))[..., None]
        btot = bcum[..., -1]
        gdec = btot[..., None] - bcum + it
        m_new = jnp.maximum(btot + m, jnp.max(gdec, axis=-1))
        decay = jnp.exp(btot + m - m_new)
        wg = jnp.exp(gdec - m_new[..., None])
        C_new = decay[..., None, None] * C + jnp.einsum('bhs,bhsd,bhse->bhde', wg, kt, vt)
        n_new = decay[..., None] * n + jnp.einsum('bhs,bhsd->bhd', wg, kt)
        return (C_new, n_new, m_new), h

    init = (jnp.zeros((Bsz, H, DH, DH), jnp.float32),
            jnp.zeros((Bsz, H, DH), jnp.float32),
            jnp.zeros((Bsz, H), jnp.float32))
    _, hs = lax.scan(step, init, (qc, kc, vc, ic, fc))
    return jnp.moveaxis(hs, 0, 2).reshape(Bsz, H, S, DH)


def mlstm_group(xc, oc, gi, gf, b_igate, b_fgate, conv_w, conv_b, w_q, w_k, w_v, hn_g, skip):
    Bsz, S, _ = xc.shape
    xconv = jax.nn.silu(causal_dwconv(xc, conv_w, conv_b))
    xh = xconv.reshape(Bsz, S, H_C, DH_C)
    xv = xc.reshape(Bsz, S, H_C, DH_C)
    q = jnp.einsum('bshd,hde->bhse', xh, w_q).astype(jnp.float32)
    k = jnp.einsum('bshd,hde->bhse', xh, w_k).astype(jnp.float32)
    v = jnp.einsum('bshd,hde->bhse', xv, w_v).astype(jnp.float32)
    logi = jnp.transpose(gi.astype(jnp.float32) + b_igate.astype(jnp.float32), (0, 2, 1))
    logf = jax.nn.log_sigmoid(jnp.transpose(gf.astype(jnp.float32) + b_fgate.astype(jnp.float32), (0, 2, 1)))
    hcell = mlstm_chunkwise(q, k, v, logi, logf)
    hcell = jnp.transpose(hcell, (0, 2, 1, 3))
    hnorm = (standardize(hcell) * hn_g.reshape(H_C, DH_C)).reshape(Bsz, S, D_C).astype(xc.dtype)
    return jax.nn.sigmoid(oc) * (hnorm + skip * xconv)


def hybrid_mixer(hn, w_in, b_igate, b_fgate, ln_a_g, ln_a_b, w_s, b_s,
                 conv_b_w, conv_b_b, gn_b_g, gn_b_b, conv_c_w, conv_c_b,
                 w_q_c, w_k_c, w_v_c, hn_c_g, skip_c, w_out):
    z = hn @ w_in
    o0 = 2 * D_A
    o1 = o0 + 2 * D_B
    o2 = o1 + D_C
    o3 = o2 + D_C
    o4 = o3 + H_C
    za = jax.nn.gelu(z[..., :o0], approximate=False)
    y_a = gmlp_spatial_gating(za[..., :D_A], za[..., D_A:], ln_a_g, ln_a_b, w_s, b_s)
    zb = z[..., o0:o1]
    y_b = conformer_conv(zb[..., :D_B], zb[..., D_B:], conv_b_w, conv_b_b, gn_b_g, gn_b_b)
    y_c = mlstm_group(z[..., o1:o2], z[..., o2:o3], z[..., o3:o4], z[..., o4:],
                      b_igate, b_fgate, conv_c_w, conv_c_b, w_q_c, w_k_c, w_v_c, hn_c_g, skip_c)
    return jnp.concatenate([y_a, y_b, y_c], axis=-1) @ w_out


def cross_attention(hn, mn, w_xq, w_xkv, w_xo):
    Bsz, S, _ = hn.shape
    M = mn.shape[1]
    q = (hn @ w_xq).reshape(Bsz, S, N_XHEADS, DH_X)
    kv = mn @ w_xkv
    k = kv[..., :D_MODEL].reshape(Bsz, M, N_XHEADS, DH_X)
    v = kv[..., D_MODEL:].reshape(Bsz, M, N_XHEADS, DH_X)
    s = jnp.einsum('bshd,bmhd->bhsm', q, k).astype(jnp.float32) * (DH_X ** -0.5)
    p = jax.nn.softmax(s, axis=-1).astype(v.dtype)
    o = jnp.einsum('bhsm,bmhd->bshd', p, v).reshape(Bsz, S, D_MODEL)
    return o @ w_xo


def conv_glu(hn, w_up, conv_f_w, conv_f_b, w_down):
    up = hn @ w_up
    gate = jax.nn.gelu(causal_dwconv(up[..., :D_FF], conv_f_w, conv_f_b), approximate=False)
    return (gate * up[..., D_FF:]) @ w_down


def setup_inputs(seed: int = 0) -> dict:
    key = jax.random.key(seed)
    ks = iter(jax.random.split(key, 48))

    def nrm(shape, scale):
        return jax.random.normal(next(ks), shape, jnp.float32) * scale

    L = DEPTH
    return {
        "x": nrm((BATCH, SEQ, D_MODEL), 1.0),
        "mem": nrm((BATCH, N_MEM, D_MODEL), 1.0),
        "norm_mix_g": 1.0 + nrm((L, D_MODEL), 0.02),
        "w_in": nrm((L, D_MODEL, D_IN), D_MODEL ** -0.5),
        "b_igate": nrm((L, H_C), 0.1),
        "b_fgate": jax.random.uniform(next(ks), (L, H_C), jnp.float32, 3.0, 6.0),
        "ln_a_g": 1.0 + nrm((L, D_A), 0.02),
        "ln_a_b": nrm((L, D_A), 0.02),
        "w_s": nrm((L, H_A, CHUNK, CHUNK), CHUNK ** -0.5),
        "b_s": 1.0 + nrm((L, H_A, CHUNK), 0.1),
        "conv_b_w": nrm((L, CONV_B, D_B), CONV_B ** -0.5),
        "conv_b_b": nrm((L, D_B), 0.02),
        "gn_b_g": 1.0 + nrm((L, D_B), 0.02),
        "gn_b_b": nrm((L, D_B), 0.02),
        "conv_c_w": nrm((L, CONV_C, D_C), CONV_C ** -0.5),
        "conv_c_b": nrm((L, D_C), 0.02),
        "w_q_c": nrm((L, H_C, DH_C, DH_C), DH_C ** -0.5),
        "w_k_c": nrm((L, H_C, DH_C, DH_C), DH_C ** -0.5),
        "w_v_c": nrm((L, H_C, DH_C, DH_C), DH_C ** -0.5),
        "hn_c_g": 1.0 + nrm((L, D_C), 0.02),
        "skip_c": 1.0 + nrm((L, D_C), 0.02),
        "w_out": nrm((L, D_MIX, D_MODEL), D_MIX ** -0.5),
        "norm_x_g": 1.0 + nrm((L, D_MODEL), 0.02),
        "norm_mem_g": 1.0 + nrm((L, D_MODEL), 0.02),
        "w_xq": nrm((L, D_MODEL, D_MODEL), D_MODEL ** -0.5),
        "w_xkv": nrm((L, D_MODEL, 2 * D_MODEL), D_MODEL ** -0.5),
        "w_xo": nrm((L, D_MODEL, D_MODEL), D_MODEL ** -0.5),
        "norm_ffn_g": 1.0 + nrm((L, D_MODEL), 0.02),
        "w_up": nrm((L, D_MODEL, 2 * D_FF), D_MODEL ** -0.5),
        "conv_f_w": nrm((L, CONV_F, D_FF), CONV_F ** -0.5),
        "conv_f_b": nrm((L, D_FF), 0.02),
        "w_down": nrm((L, D_FF, D_MODEL), D_FF ** -0.5),
        "final_g": 1.0 + nrm((D_MODEL,), 0.02),
    }


def reference(x, mem, norm_mix_g, w_in, b_igate, b_fgate, ln_a_g, ln_a_b, w_s, b_s,
              conv_b_w, conv_b_b, gn_b_g, gn_b_b, conv_c_w, conv_c_b,
              w_q_c, w_k_c, w_v_c, hn_c_g, skip_c, w_out,
              norm_x_g, norm_mem_g, w_xq, w_xkv, w_xo,
              norm_ffn_g, w_up, conv_f_w, conv_f_b, w_down, final_g):
    h = x
    for l in range(DEPTH):
        hn = rmsnorm(h, norm_mix_g[l])
        h = h + hybrid_mixer(hn, w_in[l], b_igate[l], b_fgate[l], ln_a_g[l], ln_a_b[l],
                             w_s[l], b_s[l], conv_b_w[l], conv_b_b[l], gn_b_g[l], gn_b_b[l],
                             conv_c_w[l], conv_c_b[l], w_q_c[l], w_k_c[l], w_v_c[l],
                             hn_c_g[l], skip_c[l], w_out[l])
        hn = rmsnorm(h, norm_x_g[l])
        mn = rmsnorm(mem, norm_mem_g[l])
        h = h + cross_attention(hn, mn, w_xq[l], w_xkv[l], w_xo[l])
        hn = rmsnorm(h, norm_ffn_g[l])
        h = h + conv_glu(hn, w_up[l], conv_f_w[l], conv_f_b[l], w_down[l])
    return rmsnorm(h, final_g)
```

```python
import numpy as np
import concourse.bass as bass
import concourse.mybir as mybir
from concourse.bass_utils import run_bass_kernel_spmd

F32 = mybir.dt.float32
BF16 = mybir.dt.bfloat16
ALU = mybir.AluOpType
AF = mybir.ActivationFunctionType
AX = mybir.AxisListType

D = 1024
NMEM = 256
DFF = 2816
T = 512
NJ = 4
NSLOT = 6
PIECE = 4096
PPL = 35
RMS_EPS = 1e-6
LN_EPS = 1e-5

C_NMIX, C_NX, C_NFFN = 0, 8, 16
C_CBW, C_CBB, C_GNG, C_GNB = 24, 86, 88, 90
C_CCW, C_CCB, C_SKP = 92, 108, 112
C_CFW, C_CFB = 116, 182
C_BIG, C_BFG = 204, 205
NCOL = 208
R_LNG, R_LNB, R_BS, R_HNG, R_MEMG = 0, 256, 512, 1024, 1536
NROW = 2560
K_ID, K_MLE, K_MBIG, K_BLK, K_SEL, K_ONE = 0, 128, 256, 384, 512, 1024
NCST = 1152


def _es(dt):
    return 2 if dt == BF16 else 4


class Sched:
    ENGS = ("pe", "act", "dve", "pool", "sp")

    def __init__(self, nc, ndsem=16):
        self.nc = nc
        self.q = {e: [] for e in self.ENGS}
        self.sem = {e: nc.alloc_semaphore("sem_" + e) for e in self.ENGS}
        self.cnt = {e: 0 for e in self.ENGS}
        self.dsem = [nc.alloc_semaphore("dsem%d" % i) for i in range(ndsem)]
        self.dn = 0
        self.seen = {e: {} for e in self.ENGS}
        self.rec = {}
        self.nwait = 0
        self.phase = ""
        self.labels = {e: [] for e in self.ENGS}

    def _iv(self, ap):
        dims = ap.ap
        off = int(ap.offset)
        es = _es(ap.dtype)
        sp = str(ap.space).upper()
        if "PSUM" in sp:
            return ap.tensor.name, 0, 2048
        if "DRAM" in sp:
            lo = off
            free = dims
        else:
            ps = dims[0][0]
            lo = off % ps if ps > 0 else off
            free = dims[1:]
        ext = 1
        for s, c in free:
            ext += (c - 1) * abs(s)
        return ap.tensor.name, lo * es, (lo + ext) * es

    def _deps(self, reads, writes):
        deps = {}
        for ap in reads:
            name, lo, hi = self._iv(ap)
            R = self.rec.get(name)
            if R:
                for (l, h, t) in R["w"]:
                    if l < hi and lo < h:
                        deps[t[0]] = max(deps.get(t[0], 0), t[1])
        for ap in writes:
            name, lo, hi = self._iv(ap)
            R = self.rec.get(name)
            if R:
                for (l, h, t) in R["w"]:
                    if l < hi and lo < h:
                        deps[t[0]] = max(deps.get(t[0], 0), t[1])
                for (e, l, h), t in R["r"].items():
                    if l < hi and lo < h:
                        deps[t[0]] = max(deps.get(t[0], 0), t[1])
        return deps

    def _commit(self, reads, writes, ticket):
        for ap in reads:
            name, lo, hi = self._iv(ap)
            R = self.rec.setdefault(name, {"w": [], "r": {}})
            R["r"][(ticket[0], lo, hi)] = ticket
        for ap in writes:
            name, lo, hi = self._iv(ap)
            R = self.rec.setdefault(name, {"w": [], "r": {}})
            R["w"] = [(l, h, t) for (l, h, t) in R["w"] if not (lo <= l and h <= hi)]
            R["r"] = {k: t for k, t in R["r"].items() if not (lo <= k[1] and k[2] <= hi)}
            R["w"].append((lo, hi, ticket))

    def op(self, eng, fn, reads, writes):
        deps = self._deps(reads, writes)
        waits = []
        for k, v in deps.items():
            if k == "pe" and eng == "pe":
                continue
            if self.seen[eng].get(k, 0) < v:
                self.seen[eng][k] = v
                waits.append((k, v))
        self.cnt[eng] += 1
        ticket = (eng, self.cnt[eng])
        self.q[eng].append((waits, fn, ticket))
        self.labels[eng].append((self.phase, tuple(waits)))
        self.nwait += len(waits)
        self._commit(reads, writes, ticket)

    def dma(self, out, in_, eng="sp"):
        k = self.dn % len(self.dsem)
        use = self.dn // len(self.dsem)
        self.dn += 1
        key = ("d", k)
        deps = self._deps([in_], [out])
        if use > 0:
            deps[key] = max(deps.get(key, 0), 16 * use)
        waits = []
        for kk, v in deps.items():
            if self.seen[eng].get(kk, 0) < v:
                self.seen[eng][kk] = v
                waits.append((kk, v))
        ticket = (key, 16 * (use + 1))
        self.q[eng].append((waits, lambda e: e.dma_start(out=out, in_=in_), ticket))
        self._commit([in_], [out], ticket)

    def _semof(self, k):
        return self.dsem[k[1]] if isinstance(k, tuple) else self.sem[k]

    def finish(self):
        waits = []
        n = len(self.dsem)
        for k in range(n):
            uses = (self.dn - k + n - 1) // n if self.dn > k else 0
            if uses > 0:
                waits.append((("d", k), 16 * uses))
        self.q["sp"].append((waits, None, None))

    def emit(self):
        nc = self.nc
        S = self

        def replay(name):
            def f(eng):
                for waits, fn, ticket in S.q[name]:
                    for k, v in waits:
                        eng.wait_ge(S._semof(k), v)
                    if fn is None:
                        continue
                    ins = fn(eng)
                    if isinstance(ticket[0], tuple):
                        ins.then_inc(S._semof(ticket[0]), 16)
                    else:
                        ins.then_inc(S.sem[ticket[0]], 1)
            return f

        with nc.Block() as block:
            block.tensor(replay("pe"))
            block.scalar(replay("act"))
            block.vector(replay("dve"))
            block.gpsimd(replay("pool"))
            block.sync(replay("sp"))

    def mm(self, out, lhsT, rhs, start=True, stop=True):
        self.op("pe", lambda e: e.matmul(out, lhsT, rhs, start=start, stop=stop), [lhsT, rhs], [out])

    def tr(self, out, in_, ident):
        self.op("pe", lambda e: e.transpose(out, in_, ident), [in_, ident], [out])

    def act(self, out, in_, func, bias=None, scale=None, accum_out=None):
        reads = [in_]
        writes = [out]
        kw = {}
        if bias is not None:
            kw["bias"] = bias
            if not isinstance(bias, (int, float)):
                reads.append(bias)
        if scale is not None:
            kw["scale"] = scale
            if not isinstance(scale, (int, float)):
                reads.append(scale)
        if accum_out is not None:
            kw["accum_out"] = accum_out
            writes.append(accum_out)
        self.op("act", lambda e: e.activation(out, in_, func, **kw), reads, writes)

    def tt(self, out, in0, in1, op, eng="dve"):
        self.op(eng, lambda e: e.tensor_tensor(out, in0, in1, op), [in0, in1], [out])

    def ts(self, out, in0, s1, s2, op0, op1=None, eng="dve"):
        reads = [in0]
        for s in (s1, s2):
            if s is not None and not isinstance(s, (int, float)):
                reads.append(s)
        if op1 is None:
            self.op(eng, lambda e: e.tensor_scalar(out, in0, s1, None, op0), reads, [out])
        else:
            self.op(eng, lambda e: e.tensor_scalar(out, in0, s1, s2, op0, op1), reads, [out])

    def stt(self, out, in0, scalar, in1, op0, op1):
        reads = [in0, in1]
        if not isinstance(scalar, (int, float)):
            reads.append(scalar)
        self.op("dve", lambda e: e.scalar_tensor_tensor(out, in0, scalar, in1, op0, op1), reads, [out])

    def copy(self, out, in_, eng="dve"):
        if eng == "act":
            self.act(out, in_, AF.Copy)
        else:
            self.op(eng, lambda e: e.tensor_copy(out, in_), [in_], [out])

    def memset(self, out, val, eng="pool"):
        self.op(eng, lambda e: e.memset(out, val), [], [out])


SL = 638


def build(NB, S, NL, stateio=False):
    nc = bass.Bass("TRN2", target_bir_lowering=False)
    NT = S // T
    NP = NL * PPL
    x = nc.dram_tensor("x", [NB, S, D], F32, kind="ExternalInput")
    mem = nc.dram_tensor("mem", [NB, NMEM, D], F32, kind="ExternalInput")
    wflat = nc.dram_tensor("wflat", [NP, 128, PIECE], F32, kind="ExternalInput")
    colp = nc.dram_tensor("colp", [128, NL * NCOL + 8], F32, kind="ExternalInput")
    rowp = nc.dram_tensor("rowp", [NL, NROW], F32, kind="ExternalInput")
    wsT = nc.dram_tensor("wsT", [NL, 128, 512], F32, kind="ExternalInput")
    cst = nc.dram_tensor("cst", [128, NCST], F32, kind="ExternalInput")
    out = nc.dram_tensor("out", [NB, S, D], F32, kind="ExternalOutput")
    wbf = nc.dram_tensor("wbf", [NP, 128, PIECE], BF16)
    if stateio:
        st_in = nc.dram_tensor("st_in", [128, NL * SL], F32, kind="ExternalInput")
        st_out = nc.dram_tensor("st_out", [128, NL * SL], F32, kind="ExternalOutput")

    A = nc.alloc_sbuf_tensor
    cst_t = A("cst_t", [128, NCST], F32)
    identb = A("identb", [128, 128], BF16)
    onesb = A("onesb", [128, 128], BF16)
    blkb = A("blkb", [128, 128], BF16)
    neghalf = A("neghalf", [128, 512], F32)
    epsr = A("epsr", [128, 2], F32)
    colp_t = A("colp_t", [128, NL * NCOL + 8], F32)
    nbfg = A("nbfg", [4, NL], F32)
    lnab = A("lnab", [128, NL, 512], F32)
    bsb = A("bsb", [128, NL, 2, 128], F32)
    hngb = A("hngb", [128, NL, 512], F32)
    wsTb = A("wsTb", [128, NL, 512], BF16)
    h = A("h", [128, 8, T], F32)
    hn = A("hn", [128, 8, T], BF16)
    ring = A("ring", [128, NSLOT, PIECE], BF16)
    kTx = A("kTx", [128, NL, 8, NMEM], BF16)
    Vx = A("Vx", [128, NL, 2, D], BF16)
    Cst = A("Cst", [128, NL, 4, 128], F32)
    Cbf = A("Cbf", [128, NL, 4, 128], BF16)
    nst = A("nst", [128, NL, 4], F32)
    nbf = A("nbf", [128, NL, 4], BF16)
    yhist = A("yhist", [128, NL, 2, 32 + T], BF16)
    xcb = A("xcb", [128, NL, 4, 4 + T], BF16)
    fhist = A("fhist", [128, NL, 22, 2], F32)
    Brow = A("Brow", [4, NL, 129], F32)
    Mrow = A("Mrow", [4, NL, 129], F32)
    xin = A("xin", [128, 2, D], F32)
    ostage = A("ostage", [128, 2, D], F32)
    rstd = A("rstd", [128, T], F32)
    t0 = A("t0", [128, T], F32)
    sqn = A("sqn", [128, 2, T], BF16)
    small = A("small", [128, 64], F32)
    stio = A("stio", [128, NL * SL], F32)
    NA = 12960
    arena = A("arena", [128, NA], F32)
    psb = [nc.alloc_psum_tensor("ps%d" % i, [128, 512], F32) for i in range(8)]

    S_ = Sched(nc)
    st = {"ps": 0}

    def psum():
        p = psb[st["ps"] % 8]
        st["ps"] += 1
        return p

    class Carver:
        def __init__(self):
            self.o = 0

        def f32(self, *shape):
            n = int(np.prod(shape))
            v = arena[:, self.o:self.o + n]
            self.o += n
            assert self.o <= NA, self.o
            return self._shape(v, shape)

        def bf(self, *shape):
            n = int(np.prod(shape))
            n32 = (n + 1) // 2
            v = arena[:, self.o:self.o + n32].bitcast(BF16)[:, 0:n]
            self.o += n32
            assert self.o <= NA, self.o
            return self._shape(v, shape)

        @staticmethod
        def _shape(v, shape):
            if len(shape) == 1:
                return v
            if len(shape) == 2:
                return v.rearrange("p (a b) -> p a b", b=shape[1])
            return v.rearrange("p (a b c) -> p a b c", b=shape[1], c=shape[2])

    ident = cst_t[:, K_ID:K_ID + 128]
    mle = cst_t[:, K_MLE:K_MLE + 128]
    mbig = cst_t[:, K_MBIG:K_MBIG + 128]
    onesf = cst_t[:, K_ONE:K_ONE + 128]

    def colL(l, c, n=1, p0=0, p1=128):
        return colp_t[p0:p1, l * NCOL + c:l * NCOL + c + n]

    S_.dma(cst_t[:, :], cst[:, :])
    S_.dma(colp_t[:, :], colp[:, :])
    S_.copy(identb[:, :], ident)
    S_.memset(onesb[:, :], 1.0)
    S_.copy(blkb[:, :], cst_t[:, K_BLK:K_BLK + 128])
    S_.memset(neghalf[:, :], -0.5)
    S_.memset(epsr[:, 0:1], RMS_EPS)
    S_.memset(epsr[:, 1:2], LN_EPS)
    cv = Carver()
    wtmp = cv.f32(512)
    for l in range(NL):
        S_.dma(lnab[:, l, :], rowp[l, R_LNG:R_LNG + 512].partition_broadcast(128))
        S_.dma(hngb[:, l, :], rowp[l, R_HNG:R_HNG + 512].partition_broadcast(128))
        for ncn in range(2):
            for hf in range(2):
                hh = 2 * ncn + hf
                S_.dma(bsb[hf * 64:(hf + 1) * 64, l, ncn, :],
                       rowp[l, R_BS + hh * 128:R_BS + (hh + 1) * 128].partition_broadcast(64))
        S_.dma(wtmp, wsT[l, :, :])
        S_.tt(wsTb[:, l, :].rearrange("p (a b) -> p a b", b=128),
              wtmp.rearrange("p (a b) -> p a b", b=128),
              mle.unsqueeze(1).to_broadcast([128, 4, 128]), ALU.mult)
        S_.ts(nbfg[:, l:l + 1], colL(l, C_BFG, 1, 0, 4), -1.0, None, ALU.mult)

    cv = Carver()
    stage = [cv.f32(PIECE) for _ in range(3)]
    hb = h[:, :, :].rearrange("p a b -> p (a b)").bitcast(BF16)
    obf = [hb[:, 0:PIECE], hb[:, PIECE:2 * PIECE], hn[:, :, :].rearrange("p a b -> p (a b)")]
    cast_eng = ["dve", "pool", "act"]
    for i in range(NP):
        S_.dma(stage[i % 3], wflat[i, :, :])
        S_.copy(obf[i % 3], stage[i % 3], eng=cast_eng[i % 3])
        S_.dma(wbf[i, :, :], obf[i % 3])

    order = []
    for b in range(NB):
        for l in range(NL):
            order += [l * PPL + k for k in range(4)]
        for ti in range(NT):
            for l in range(NL):
                order += [l * PPL + k for k in range(4, PPL)]
    ws = {"issued": 0, "pos": 0}

    def wnext(expect):
        while ws["issued"] < min(len(order), ws["pos"] + NSLOT):
            i = ws["issued"]
            S_.dma(ring[:, i % NSLOT, :], wbf[order[i], :, :])
            ws["issued"] += 1
        assert order[ws["pos"]] == expect, (order[ws["pos"]], expect)
        slot = ws["pos"] % NSLOT
        ws["pos"] += 1
        return ring[:, slot, :]

    def w3(v, n):
        return v.rearrange("p (a b) -> p a b", b=n)

    def rms_stats(src):
        ps = psum()
        for c in range(8):
            S_.act(sqn[:, c % 2, :], src[:, c, :], AF.Square)
            S_.mm(ps[:, :], onesb[:, :], sqn[:, c % 2, :], start=(c == 0), stop=(c == 7))
        S_.act(t0[:, :], ps[:, :], AF.Ln, bias=epsr[:, 0:1], scale=1.0 / D)
        S_.act(rstd[:, :], t0[:, :], AF.Exp, scale=-0.5)

    def norm_to_hn(gcol):
        rms_stats(h)
        for c in range(8):
            S_.stt(hn[:, c, :], h[:, c, :], gcol[:, c:c + 1], rstd[:, :], ALU.mult, ALU.mult)

    def proj_fm(W, col0, rhs_t, nk, ps_ap):
        for kc in range(nk):
            S_.mm(ps_ap, W[:, kc, col0:col0 + 128], rhs_t[:, kc, :], start=(kc == 0), stop=(kc == nk - 1))

    def resid_proj(pid0, src):
        for half in range(2):
            W = w3(wnext(pid0 + half), 512)
            for n4 in range(4):
                ncn = half * 4 + n4
                ps = psum()
                proj_fm(W, n4 * 128, src, 8, ps[:, :])
                S_.tt(h[:, ncn, :], h[:, ncn, :], ps[:, :], ALU.add)

    def rsqrt_small(out, in_, eps, n):
        S_.ts(out, in_, eps, None, ALU.add)
        S_.tt(out, out, neghalf[0:out.shape[0], 0:n], ALU.pow, eng="pool")

    for b in range(NB):
        for l in range(NL):
            cv = Carver()
            memt = cv.f32(2, D)
            memg = cv.f32(D)
            mnb = cv.bf(2, D)
            mnT = cv.bf(8, NMEM)
            junk = cv.f32(D)
            S_.dma(memg, rowp[l, R_MEMG:R_MEMG + D].partition_broadcast(128))
            ss = small[:, 0:2]
            rs2 = small[:, 2:4]
            for mc in range(2):
                S_.dma(memt[:, mc, :], mem[b, mc * 128:(mc + 1) * 128, :])
                S_.act(junk, memt[:, mc, :], AF.Square, accum_out=ss[:, mc:mc + 1])
            S_.ts(rs2, ss, 1.0 / D, RMS_EPS, ALU.mult, ALU.add)
            S_.tt(rs2, rs2, neghalf[:, 0:2], ALU.pow, eng="pool")
            for mc in range(2):
                S_.stt(mnb[:, mc, :], memt[:, mc, :], rs2[:, mc:mc + 1], memg, ALU.mult, ALU.mult)
                for half in range(2):
                    ps = psum()
                    pv = ps[:, :].bitcast(BF16)
                    for k4 in range(4):
                        kc = half * 4 + k4
                        S_.tr(pv[:, k4 * 128:(k4 + 1) * 128], mnb[:, mc, kc * 128:(kc + 1) * 128], identb[:, :])
                    S_.copy(mnT[:, half * 4:half * 4 + 4, mc * 128:(mc + 1) * 128],
                            pv[:, 0:512].rearrange("p (a b) -> p a b", b=128), eng="act")
            for pk in range(2):
                W = w3(wnext(l * PPL + pk), 512)
                for d4 in range(4):
                    dch = pk * 4 + d4
                    ps = psum()
                    proj_fm(W, d4 * 128, mnT, 8, ps[:, 0:NMEM])
                    S_.copy(kTx[:, l, dch, :], ps[:, 0:NMEM], eng="act")
            for pv_ in range(2):
                W = w3(wnext(l * PPL + 2 + pv_), 512)
                for mc in range(2):
                    ps = psum()
                    for kc in range(8):
                        S_.mm(ps[:, :], mnT[:, kc, mc * 128:(mc + 1) * 128], W[:, kc, :], start=(kc == 0), stop=(kc == 7))
                    S_.copy(Vx[:, l, mc, pv_ * 512:(pv_ + 1) * 512], ps[:, :], eng="act")
        S_.memset(Cst[:, :, :, :], 0.0)
        S_.memset(Cbf[:, :, :, :], 0.0)
        S_.memset(nst[:, :, :], 0.0)
        S_.memset(nbf[:, :, :], 0.0)
        S_.memset(yhist[:, :, :, 0:32], 0.0)
        S_.memset(xcb[:, :, :, 0:4], 0.0)
        S_.memset(fhist[:, :, :, :], 0.0)
        S_.memset(Brow[:, :, 0:1], 0.0)
        S_.memset(Mrow[:, :, 0:1], 0.0)
        if stateio:
            S_.dma(stio[:, :], st_in[:, :])
            for l in range(NL):
                o = l * SL
                S_.copy(Cst[:, l, :, :], stio[:, o:o + 512].rearrange("p (a b) -> p a b", b=128))
                S_.copy(nst[:, l, :], stio[:, o + 512:o + 516])
                S_.copy(yhist[:, l, :, 2:32], stio[:, o + 516:o + 576].rearrange("p (a b) -> p a b", b=30))
                S_.copy(xcb[:, l, :, 0:4], stio[:, o + 576:o + 592].rearrange("p (a b) -> p a b", b=4))
                S_.copy(fhist[:, l, :, :], stio[:, o + 592:o + 636].rearrange("p (a b) -> p a b", b=2))
                S_.copy(Brow[:, l, 0:1], stio[0:4, o + 636:o + 637])
                S_.copy(Mrow[:, l, 0:1], stio[0:4, o + 637:o + 638])
                S_.copy(Cbf[:, l, :, :], Cst[:, l, :, :], eng="pool")
                S_.copy(nbf[:, l, :], nst[:, l, :], eng="pool")

        for ti in range(NT):
            tok0 = ti * T
            S_.phase = "xload"
            for j in range(NJ):
                S_.dma(xin[:, j % 2, :], x[b, tok0 + j * 128:tok0 + (j + 1) * 128, :])
                for half in range(2):
                    ps = psum()
                    for c4 in range(4):
                        c = half * 4 + c4
                        S_.tr(ps[:, c4 * 128:(c4 + 1) * 128], xin[:, j % 2, c * 128:(c + 1) * 128], ident)
                    S_.copy(h[:, half * 4:half * 4 + 4, j * 128:(j + 1) * 128],
                            ps[:, :].rearrange("p (a b) -> p a b", b=128), eng="act")

            for l in range(NL):
                pid = l * PPL
                S_.phase = "mixnorm"
                norm_to_hn(colL(l, C_NMIX, 8))
                cv = Carver()
                ycat = cv.bf(8, T)
                xconv = cv.bf(4, T)
                sgo = cv.bf(4, T)
                logi_row = cv.f32(T)
                sp_row = cv.f32(T)
                mark = cv.o
                S_.phase = "A"
                u = cv.bf(2, T)
                vn = cv.bf(4, 256)
                vg = [cv.f32(256) for _ in range(NJ)]
                W = w3(wnext(pid + 4), 512)
                for ncn in range(2):
                    ps = psum()
                    proj_fm(W, ncn * 128, hn, 8, ps[:, :])
                    S_.act(u[:, ncn, :], ps[:, :], AF.Gelu)
                for j in range(NJ):
                    js = slice(j * 128, (j + 1) * 128)
                    ps = psum()
                    for kc in range(8):
                        S_.mm(ps[:, 0:256], hn[:, kc, js], W[:, kc, 256:512], start=(kc == 0), stop=(kc == 7))
                    S_.act(vg[j], ps[:, 0:256], AF.Gelu)
                for j in range(NJ):
                    v_ = vg[j]
                    so = 8 + (j % 2) * 12
                    st6 = small[:, so:so + 6]
                    mv = small[:, so + 6:so + 8]
                    rsa = small[:, so + 8:so + 9]
                    S_.op("dve", lambda e, o=st6, i=v_: e.bn_stats(o, i), [v_], [st6])
                    S_.op("dve", lambda e, o=mv, i=st6: e.bn_aggr(o, i), [st6], [mv])
                    rsqrt_small(rsa, mv[:, 1:2], LN_EPS, 1)
                    S_.ts(v_, v_, mv[:, 0:1], rsa, ALU.subtract, ALU.mult)
                    S_.tt(v_, v_, lnab[:, l, 0:256], ALU.mult)
                    S_.tt(vn[:, j, :], v_, lnab[:, l, 256:512], ALU.add)
                cv.o = mark
                colsJ = [cv.f32(20) for _ in range(NJ)]
                wTJ = [cv.f32(4, 128) for _ in range(NJ)]
                r2 = cv.o
                a_R = cv.f32(4, 128)
                rtmp = cv.f32(128)
                arg = cv.f32(4, 128)
                r3 = cv.o
                cv.o = r2
                sgB = cv.f32(T)
                tmpa = cv.f32(2, 128)
                cv.o = r3
                y2 = cv.f32(T)
                y2b = cv.bf(T)
                dB = cv.f32(T)
                sqd = cv.bf(T)
                t1 = cv.f32(T)
                tmpB = cv.f32(T)
                diag = cv.bf(31, 128)
                cv.o = r3
                cvt = cv.f32(T)
                sgc = cv.f32(T)
                cv.o = r3
                qTb = cv.bf(4, 128)
                kTb = cv.bf(4, 128)
                kwb = cv.bf(4, 128)
                vtb = cv.bf(4, 128)
                scT = cv.bf(4, 128)
                n1 = cv.f32(4, 128)
                hs = cv.f32(4, 128)
                hnt = cv.f32(4, 128)
                t2 = n1
                S_.phase = "B"
                W = w3(wnext(pid + 5), 512)
                for c in range(2):
                    psa = psum()
                    proj_fm(W, c * 128, hn, 8, psa[:, :])
                    psg = psum()
                    proj_fm(W, 256 + c * 128, hn, 8, psg[:, :])
                    S_.act(sgB, psg[:, :], AF.Sigmoid)
                    S_.tt(yhist[:, l, c, 32:32 + T], psa[:, :], sgB, ALU.mult)
                for jt in range(31):
                    S_.act(diag[:, jt, :], identb[:, :], AF.Copy, scale=colL(l, C_CBW + jt * 2 + 0))
                S_.phase = "Cproj"
                W = w3(wnext(pid + 6), 512)
                for hh in range(4):
                    ps = psum()
                    proj_fm(W, hh * 128, hn, 8, ps[:, :])
                    S_.copy(xcb[:, l, hh, 4:4 + T], ps[:, :], eng="act")
                W = w3(wnext(pid + 7), 512)
                for hh in range(4):
                    ps = psum()
                    proj_fm(W, hh * 128, hn, 8, ps[:, :])
                    S_.act(sgo[:, hh, :], ps[:, :], AF.Sigmoid)
                W = w3(wnext(pid + 8)[:, 0:64], 8)
                psi = psum()
                for kc in range(8):
                    S_.mm(psi[0:4, :], W[:, kc, 0:4], hn[:, kc, :], start=(kc == 0), stop=(kc == 7))
                psf = psum()
                for kc in range(8):
                    S_.mm(psf[0:4, :], W[:, kc, 4:8], hn[:, kc, :], start=(kc == 0), stop=(kc == 7))
                for hh in range(4):
                    S_.ts(cvt, xcb[:, l, hh, 1:1 + T], colL(l, C_CCW + hh), colL(l, C_CCB + hh), ALU.mult, ALU.add)
                    for jt in range(1, 4):
                        S_.stt(cvt, xcb[:, l, hh, 1 + jt:1 + jt + T], colL(l, C_CCW + jt * 4 + hh), cvt, ALU.mult, ALU.add)
                    S_.act(sgc, cvt, AF.Sigmoid)
                    S_.tt(xconv[:, hh, :], cvt, sgc, ALU.mult)
                S_.phase = "A"
                for j in range(NJ):
                    js = slice(j * 128, (j + 1) * 128)
                    ps = psum()
                    for hh in range(4):
                        p0 = (hh % 2) * 64
                        S_.mm(ps[p0:p0 + 64, (hh // 2) * 128:(hh // 2 + 1) * 128],
                              vn[:, j, hh * 64:(hh + 1) * 64], wsTb[:, l, hh * 128:(hh + 1) * 128])
                    S_.tt(tmpa, ps[:, 0:256].rearrange("p (a b) -> p a b", b=128), bsb[:, l, :, :], ALU.add)
                    S_.tt(ycat[:, 0:2, js], tmpa, u[:, 0:2, js], ALU.mult)
                S_.phase = "Cgate"
                S_.act(logi_row[0:4, :], psi[0:4, :], AF.Identity, bias=colL(l, C_BIG, 1, 0, 4))
                S_.act(sp_row[0:4, :], psf[0:4, :], AF.Exp, bias=nbfg[:, l:l + 1], scale=-1.0)
                S_.act(sp_row[0:4, :], sp_row[0:4, :], AF.Ln, bias=1.0)
                for j in range(NJ):
                    js = slice(j * 128, (j + 1) * 128)
                    cols = colsJ[j]
                    wT = wTJ[j]
                    Bn = Brow[:, l, 1:129]
                    Mn = Mrow[:, l, 1:129]
                    S_.op("dve", lambda e, o=Bn, d1=sp_row[0:4, js], i0=Brow[:, l, 0:1]:
                          e.tensor_tensor_scan(o, onesf[0:4, :], d1, i0, ALU.mult, ALU.subtract),
                          [onesf[0:4, :], sp_row[0:4, js], Brow[:, l, 0:1]], [Bn])
                    S_.tt(a_R[0:4, 0, :], logi_row[0:4, js], Bn, ALU.subtract)
                    S_.op("dve", lambda e, o=Mn, d1=a_R[0:4, 0, :], i0=Mrow[:, l, 0:1]:
                          e.tensor_tensor_scan(o, onesf[0:4, :], d1, i0, ALU.mult, ALU.max),
                          [onesf[0:4, :], a_R[0:4, 0, :], Mrow[:, l, 0:1]], [Mn])
                    S_.act(a_R[0:4, 1, :], Mn, AF.Exp, bias=Mrow[:, l, 0:1], scale=-1.0)
                    S_.tt(rtmp[0:4, :], Bn, Mn, ALU.add)
                    S_.act(a_R[0:4, 2, :], rtmp[0:4, :], AF.Exp, scale=-1.0)
                    negM = small[0:4, 20:21]
                    S_.ts(negM, Mrow[:, l, 128:129], -1.0, None, ALU.mult)
                    S_.act(a_R[0:4, 3, :], a_R[0:4, 0, :], AF.Exp, bias=negM)
                    dg = small[0:4, 24:28]
                    S_.ts(dg, ident[0:4, 0:4], a_R[0:4, 1, 127:128], None, ALU.mult)
                    psc = psum()
                    for q in range(4):
                        S_.tr(psc[:, q * 4:(q + 1) * 4], a_R[0:4, q, :], ident[0:4, 0:4])
                    S_.mm(psc[:, 16:20], onesf[0:4, :], dg)
                    S_.copy(cols, psc[:, 0:20])
                    psM = psum()
                    for hh in range(4):
                        S_.mm(psM[:, hh * 128:(hh + 1) * 128], cst_t[0:4, K_SEL + hh * 128:K_SEL + (hh + 1) * 128], Mn)
                    S_.tt(arg, psM[:, :].rearrange("p (a b) -> p a b", b=128),
                          mbig.unsqueeze(1).to_broadcast([128, 4, 128]), ALU.add)
                    S_.copy(Brow[:, l, 0:1], Brow[:, l, 128:129], eng="pool")
                    S_.copy(Mrow[:, l, 0:1], Mrow[:, l, 128:129], eng="pool")
                    for hh in range(4):
                        S_.act(wT[:, hh, :], arg[:, hh, :], AF.Exp, bias=cols[:, hh:hh + 1], scale=-1.0)
                S_.phase = "B"
                for c in range(2):
                    if c == 1:
                        for jt in range(31):
                            S_.act(diag[:, jt, :], identb[:, :], AF.Copy, scale=colL(l, C_CBW + jt * 2 + c))
                    psc31 = psum()
                    for jt in range(31):
                        S_.mm(psc31[:, :], diag[:, jt, :], yhist[:, l, c, 2 + jt:2 + jt + T], start=(jt == 0), stop=(jt == 30))
                    S_.copy(yhist[:, l, c, 0:32], yhist[:, l, c, T:T + 32], eng="pool")
                    S_.act(y2, psc31[:, :], AF.Identity, bias=colL(l, C_CBB + c))
                    S_.act(y2b, psc31[:, :], AF.Identity, bias=colL(l, C_CBB + c))
                    psm = psum()
                    S_.mm(psm[:, :], blkb[:, :], y2b)
                    S_.tt(dB, y2, psm[:, :], ALU.subtract)
                    S_.act(sqd, dB, AF.Square)
                    psv = psum()
                    S_.mm(psv[:, :], blkb[:, :], sqd)
                    S_.act(t1, psv[:, :], AF.Ln, bias=epsr[:, 1:2])
                    S_.act(t1, t1, AF.Exp, scale=-0.5)
                    S_.stt(tmpB, dB, colL(l, C_GNG + c), t1, ALU.mult, ALU.mult)
                    S_.act(y2, tmpB, AF.Sigmoid, bias=colL(l, C_GNB + c))
                    S_.stt(ycat[:, 2 + c, :], tmpB, colL(l, C_GNB + c), y2, ALU.add, ALU.mult)
                Wq = wnext(pid + 9)[:, 0:1536].rearrange("p (a b c) -> p a b c", b=4, c=128)
                S_.phase = "Cloop"
                for j in range(NJ):
                    js = slice(j * 128, (j + 1) * 128)
                    psq = psum()
                    for hh in range(4):
                        S_.mm(psq[:, hh * 128:(hh + 1) * 128], Wq[:, 0, hh, :], xconv[:, hh, js])
                    S_.act(qTb, psq[:, :].rearrange("p (a b) -> p a b", b=128), AF.Copy, scale=128.0 ** -0.5)
                    psk = psum()
                    for hh in range(4):
                        S_.mm(psk[:, hh * 128:(hh + 1) * 128], Wq[:, 1, hh, :], xconv[:, hh, js])
                    S_.copy(kTb, psk[:, :].rearrange("p (a b) -> p a b", b=128), eng="act")
                    pskt = psum()
                    for hh in range(4):
                        S_.mm(pskt[:, hh * 128:(hh + 1) * 128], xconv[:, hh, js], Wq[:, 1, hh, :])
                    psvt = psum()
                    for hh in range(4):
                        S_.mm(psvt[:, hh * 128:(hh + 1) * 128], xcb[:, l, hh, 4 + j * 128:4 + (j + 1) * 128], Wq[:, 2, hh, :])
                    S_.copy(vtb, psvt[:, :].rearrange("p (a b) -> p a b", b=128), eng="act")
                    pss = psum()
                    for hh in range(4):
                        S_.mm(pss[:, hh * 128:(hh + 1) * 128], kTb[:, hh, :], qTb[:, hh, :])
                    psP1 = psum()
                    psD = psum()
                    for hh in range(4):
                        S_.mm(psP1[:, hh * 128:(hh + 1) * 128], qTb[:, hh, :], Cbf[:, l, hh, :])
                        S_.mm(psD[:, hh:hh + 1], qTb[:, hh, :], nbf[:, l, hh:hh + 1])
                    cols = colsJ[j]
                    wT = wTJ[j]
                    S_.tt(kwb, pskt[:, :].rearrange("p (a b) -> p a b", b=128),
                          cols[:, 12:16].unsqueeze(2).to_broadcast([128, 4, 128]), ALU.mult)
                    S_.tt(scT, pss[:, :].rearrange("p (a b) -> p a b", b=128), wT, ALU.mult)
                    psP2 = psum()
                    psD_b = psum()
                    for hh in range(4):
                        S_.mm(psP2[:, hh * 128:(hh + 1) * 128], scT[:, hh, :], vtb[:, hh, :])
                        S_.mm(psD_b[:, hh:hh + 1], scT[:, hh, :], onesb[:, 0:1])
                    den = small[:, 28:32]
                    rden = small[:, 32:36]
                    S_.tt(den, psD[:, 0:4], cols[:, 4:8], ALU.mult)
                    S_.tt(den, den, psD_b[:, 0:4], ALU.add)
                    S_.stt(den, den, -1.0, den, ALU.mult, ALU.max)
                    S_.tt(den, den, cols[:, 8:12], ALU.max)
                    S_.op("dve", lambda e, o=rden, i=den: e.reciprocal(o, i), [den], [rden])
                    for hh in range(4):
                        S_.act(n1[:, hh, :], psP1[:, hh * 128:(hh + 1) * 128], AF.Copy, scale=cols[:, 4 + hh:5 + hh])
                    S_.tt(hs, n1, psP2[:, :].rearrange("p (a b) -> p a b", b=128), ALU.add)
                    S_.tt(hs, hs, rden.unsqueeze(2).to_broadcast([128, 4, 128]), ALU.mult, eng="pool")
                    st24 = small[:, 36:60].rearrange("p (a b) -> p a b", b=6)
                    mv8 = small[:, 0:8].rearrange("p (a b) -> p a b", b=2)
                    for hh in range(4):
                        S_.op("dve", lambda e, o=st24[:, hh, :], i=hs[:, hh, :]: e.bn_stats(o, i), [hs[:, hh, :]], [st24[:, hh, :]])
                        S_.op("dve", lambda e, o=mv8[:, hh, :], i=st24[:, hh, :]: e.bn_aggr(o, i), [st24[:, hh, :]], [mv8[:, hh, :]])
                    rsc = small[:, 60:64]
                    S_.ts(rsc, mv8[:, :, 1], LN_EPS, None, ALU.add)
                    S_.tt(rsc, rsc, neghalf[:, 0:4], ALU.pow, eng="pool")
                    for hh in range(4):
                        S_.ts(hnt[:, hh, :], hs[:, hh, :], mv8[:, hh, 0:1], rsc[:, hh:hh + 1], ALU.subtract, ALU.mult)
                    S_.tt(hnt, hnt, hngb[:, l, :].rearrange("p (a b) -> p a b", b=128), ALU.mult)
                    psT = psum()
                    for hh in range(4):
                        S_.tr(psT[:, hh * 128:(hh + 1) * 128], hnt[:, hh, :], ident)
                    for hh in range(4):
                        S_.stt(t2[:, hh, :], xconv[:, hh, js], colL(l, C_SKP + hh), psT[:, hh * 128:(hh + 1) * 128], ALU.mult, ALU.add)
                    S_.tt(ycat[:, 4:8, js], t2, sgo[:, :, js], ALU.mult, eng="pool")
                    psU = psum()
                    psD2 = psum()
                    for hh in range(4):
                        S_.mm(psU[:, hh * 128:(hh + 1) * 128], kwb[:, hh, :], vtb[:, hh, :])
                        S_.mm(psD2[:, hh:hh + 1], kwb[:, hh, :], onesb[:, 0:1])
                    for hh in range(4):
                        S_.stt(Cst[:, l, hh, :], Cst[:, l, hh, :], cols[:, 16 + hh:17 + hh], psU[:, hh * 128:(hh + 1) * 128], ALU.mult, ALU.add)
                    S_.tt(nst[:, l, :], nst[:, l, :], cols[:, 16:20], ALU.mult)
                    S_.tt(nst[:, l, :], nst[:, l, :], psD2[:, 0:4], ALU.add)
                    S_.copy(Cbf[:, l, :, :], Cst[:, l, :, :], eng="pool")
                    S_.copy(nbf[:, l, :], nst[:, l, :], eng="pool")
                for hh in range(4):
                    S_.copy(xcb[:, l, hh, 0:4], xcb[:, l, hh, T:T + 4], eng="pool")
                S_.phase = "outproj"
                resid_proj(pid + 10, ycat)

                S_.phase = "xattn"
                norm_to_hn(colL(l, C_NX, 8))
                cv = Carver()
                qx = cv.bf(8, T)
                oT = cv.bf(8, T)
                pT = cv.bf(8, T)
                pexp2 = [cv.bf(4, NMEM) for _ in range(2)]
                pn2 = [cv.bf(4, NMEM) for _ in range(2)]
                for half in range(2):
                    W = w3(wnext(pid + 12 + half), 512)
                    for n4 in range(4):
                        ps = psum()
                        proj_fm(W, n4 * 128, hn, 8, ps[:, :])
                        S_.copy(qx[:, half * 4 + n4, :], ps[:, :], eng="act")
                def qk_scores(j):
                    js = slice(j * 128, (j + 1) * 128)
                    pss2 = [psum(), psum()]
                    for hh in range(4):
                        pp = pss2[hh // 2]
                        o0 = (hh % 2) * NMEM
                        for dc in range(2):
                            S_.mm(pp[:, o0:o0 + NMEM], qx[:, hh * 2 + dc, js], kTx[:, l, hh * 2 + dc, :], start=(dc == 0), stop=(dc == 1))
                    return pss2

                pend = qk_scores(0)
                for j in range(NJ):
                    js = slice(j * 128, (j + 1) * 128)
                    so = (j % 2) * 16
                    mx = small[:, so + 0:so + 4]
                    nmx = small[:, so + 4:so + 8]
                    ssum = small[:, so + 8:so + 12]
                    rsx = small[:, so + 12:so + 16]
                    pexp = pexp2[j % 2]
                    pn = pn2[j % 2]
                    pss2 = pend
                    if j + 1 < NJ:
                        pend = qk_scores(j + 1)
                    for pr in range(2):
                        S_.op("dve", lambda e, o=mx[:, pr * 2:pr * 2 + 2], i=pss2[pr][:, :].rearrange("p (a b) -> p a b", b=NMEM):
                              e.tensor_reduce(o, i, AX.X, ALU.max),
                              [pss2[pr][:, :]], [mx[:, pr * 2:pr * 2 + 2]])
                    S_.ts(nmx, mx, -1.0 / 16.0, None, ALU.mult)
                    for hh in range(4):
                        pp = pss2[hh // 2]
                        o0 = (hh % 2) * NMEM
                        S_.act(pexp[:, hh, :], pp[:, o0:o0 + NMEM], AF.Exp, bias=nmx[:, hh:hh + 1], scale=1.0 / 16.0,
                               accum_out=ssum[:, hh:hh + 1])
                    S_.op("dve", lambda e, o=rsx, i=ssum: e.reciprocal(o, i), [ssum], [rsx])
                    S_.tt(pn, pexp, rsx.unsqueeze(2).to_broadcast([128, 4, NMEM]), ALU.mult)
                    ps = psum()
                    pv = ps[:, :].bitcast(BF16)
                    for hh in range(4):
                        for mc in range(2):
                            q_ = hh * 2 + mc
                            S_.tr(pv[:, q_ * 128:(q_ + 1) * 128], pn[:, hh, mc * 128:(mc + 1) * 128], identb[:, :])
                    S_.copy(pT[:, :, js], pv[:, :].rearrange("p (a b) -> p a b", b=128), eng="act")
                for hh in range(4):
                    for dc in range(2):
                        ps = psum()
                        for mc in range(2):
                            S_.mm(ps[:, :], Vx[:, l, mc, hh * 256 + dc * 128:hh * 256 + (dc + 1) * 128], pT[:, hh * 2 + mc, :],
                                  start=(mc == 0), stop=(mc == 1))
                        S_.copy(oT[:, hh * 2 + dc, :], ps[:, :], eng="act")
                resid_proj(pid + 14, oT)

                S_.phase = "ffn"
                norm_to_hn(colL(l, C_NFFN, 8))
                cv = Carver()
                actb = cv.bf(22, T)
                gx = [cv.f32(T + 2) for _ in range(2)]
                acc = [cv.f32(T) for _ in range(2)]
                gl = [cv.f32(T) for _ in range(2)]
                for p in range(11):
                    W = w3(wnext(pid + 16 + p), 512)
                    for cc in range(2):
                        c = 2 * p + cc
                        psg = psum()
                        proj_fm(W, cc * 128, hn, 8, psg[:, :])
                        psv = psum()
                        proj_fm(W, 256 + cc * 128, hn, 8, psv[:, :])
                        g_ = gx[c % 2]
                        a_ = acc[c % 2]
                        l_ = gl[c % 2]
                        S_.copy(g_[:, 0:2], fhist[:, l, c, :], eng="pool")
                        S_.copy(g_[:, 2:T + 2], psg[:, :], eng="act")
                        S_.copy(fhist[:, l, c, :], g_[:, T:T + 2], eng="pool")
                        S_.act(a_, psg[:, :], AF.Identity, bias=colL(l, C_CFB + c), scale=colL(l, C_CFW + 2 * 22 + c))
                        S_.stt(a_, g_[:, 1:T + 1], colL(l, C_CFW + 1 * 22 + c), a_, ALU.mult, ALU.add)
                        S_.stt(a_, g_[:, 0:T], colL(l, C_CFW + c), a_, ALU.mult, ALU.add)
                        S_.act(l_, a_, AF.Gelu)
                        S_.tt(actb[:, c, :], l_, psv[:, :], ALU.mult)
                for ncn in range(8):
                    W = w3(wnext(pid + 27 + ncn)[:, 0:22 * 128], 128)
                    ps = psum()
                    for kc in range(22):
                        S_.mm(ps[:, :], W[:, kc, :], actb[:, kc, :], start=(kc == 0), stop=(kc == 21))
                    S_.tt(h[:, ncn, :], h[:, ncn, :], ps[:, :], ALU.add)

            S_.phase = "final"
            rms_stats(h)
            cv = Carver()
            of = cv.f32(8, T)
            fg = colp_t[:, NL * NCOL:NL * NCOL + 8]
            for c in range(8):
                S_.stt(of[:, c, :], h[:, c, :], fg[:, c:c + 1], rstd[:, :], ALU.mult, ALU.mult)
            for j in range(NJ):
                js = slice(j * 128, (j + 1) * 128)
                for half in range(2):
                    ps = psum()
                    for c4 in range(4):
                        c = half * 4 + c4
                        S_.tr(ps[:, c4 * 128:(c4 + 1) * 128], of[:, c, js], ident)
                    S_.copy(ostage[:, j % 2, half * 512:(half + 1) * 512], ps[:, :], eng="act")
                S_.dma(out[b, tok0 + j * 128:tok0 + (j + 1) * 128, :], ostage[:, j % 2, :])

    if stateio:
        S_.memset(stio[:, :], 0.0)
        for l in range(NL):
            o = l * SL
            S_.copy(stio[:, o:o + 512].rearrange("p (a b) -> p a b", b=128), Cst[:, l, :, :])
            S_.copy(stio[:, o + 512:o + 516], nst[:, l, :])
            S_.copy(stio[:, o + 516:o + 576].rearrange("p (a b) -> p a b", b=30), yhist[:, l, :, 2:32])
            S_.copy(stio[:, o + 576:o + 592].rearrange("p (a b) -> p a b", b=4), xcb[:, l, :, 0:4])
            S_.copy(stio[:, o + 592:o + 636].rearrange("p (a b) -> p a b", b=2), fhist[:, l, :, :])
            S_.copy(stio[0:4, o + 636:o + 637], Brow[:, l, 0:1])
            S_.copy(stio[0:4, o + 637:o + 638], Mrow[:, l, 0:1])
        S_.dma(st_out[:, :], stio[:, :])
    assert ws["pos"] == len(order), (ws["pos"], len(order))
    S_.finish()
    S_.emit()
    return nc, S_


def _pad(a):
    a = np.ascontiguousarray(a, dtype=np.float32).reshape(128, -1)
    o = np.zeros((128, PIECE), np.float32)
    o[:, :a.shape[1]] = a
    return o


def _k3(w):
    K, N = w.shape
    return w.reshape(K // 128, 128, N).transpose(1, 0, 2)


def _pieces(inp, l):
    P = []
    wkv = _k3(inp["w_xkv"][l])
    for k in range(4):
        P.append(_pad(wkv[:, :, k * 512:(k + 1) * 512]))
    win = _k3(inp["w_in"][l])
    for k in range(4):
        P.append(_pad(win[:, :, k * 512:(k + 1) * 512]))
    P.append(_pad(win[:, :, 2048:2056]))
    qkv = np.stack([inp["w_q_c"][l], inp["w_k_c"][l], inp["w_v_c"][l]], 0)
    P.append(_pad(qkv.transpose(2, 0, 1, 3)))
    for name in ("w_out", "w_xq", "w_xo"):
        w = _k3(inp[name][l])
        for k in range(2):
            P.append(_pad(w[:, :, k * 512:(k + 1) * 512]))
    wup = _k3(inp["w_up"][l])
    for p in range(11):
        g = wup[:, :, p * 256:(p + 1) * 256]
        v = wup[:, :, DFF + p * 256:DFF + (p + 1) * 256]
        P.append(_pad(np.concatenate([g, v], axis=2)))
    wd = _k3(inp["w_down"][l])
    for n in range(8):
        P.append(_pad(wd[:, :, n * 128:(n + 1) * 128]))
    assert len(P) == PPL
    return P


def _col(v):
    v = np.asarray(v, np.float32)
    return v.reshape(-1, 128).T


def host_layout(inp, NL):
    wflat = np.stack([p for l in range(NL) for p in _pieces(inp, l)], 0)
    colp = np.zeros((128, NL * NCOL + 8), np.float32)
    rowp = np.zeros((NL, NROW), np.float32)
    for l in range(NL):
        o = l * NCOL
        colp[:, o + C_NMIX:o + C_NMIX + 8] = _col(inp["norm_mix_g"][l])
        colp[:, o + C_NX:o + C_NX + 8] = _col(inp["norm_x_g"][l])
        colp[:, o + C_NFFN:o + C_NFFN + 8] = _col(inp["norm_ffn_g"][l])
        cbw = inp["conv_b_w"][l]
        for jt in range(31):
            colp[:, o + C_CBW + jt * 2:o + C_CBW + jt * 2 + 2] = _col(cbw[jt])
        colp[:, o + C_CBB:o + C_CBB + 2] = _col(inp["conv_b_b"][l])
        colp[:, o + C_GNG:o + C_GNG + 2] = _col(inp["gn_b_g"][l])
        colp[:, o + C_GNB:o + C_GNB + 2] = _col(inp["gn_b_b"][l])
        ccw = inp["conv_c_w"][l]
        for jt in range(4):
            colp[:, o + C_CCW + jt * 4:o + C_CCW + jt * 4 + 4] = _col(ccw[jt])
        colp[:, o + C_CCB:o + C_CCB + 4] = _col(inp["conv_c_b"][l])
        colp[:, o + C_SKP:o + C_SKP + 4] = _col(inp["skip_c"][l])
        cfw = inp["conv_f_w"][l]
        for jt in range(3):
            colp[:, o + C_CFW + jt * 22:o + C_CFW + (jt + 1) * 22] = _col(cfw[jt])
        colp[:, o + C_CFB:o + C_CFB + 22] = _col(inp["conv_f_b"][l])
        colp[0:4, o + C_BIG] = inp["b_igate"][l]
        colp[0:4, o + C_BFG] = inp["b_fgate"][l]
        rowp[l, R_LNG:R_LNG + 256] = inp["ln_a_g"][l]
        rowp[l, R_LNB:R_LNB + 256] = inp["ln_a_b"][l]
        rowp[l, R_BS:R_BS + 512] = np.asarray(inp["b_s"][l]).reshape(-1)
        rowp[l, R_HNG:R_HNG + 512] = inp["hn_c_g"][l]
        rowp[l, R_MEMG:R_MEMG + D] = inp["norm_mem_g"][l]
    colp[:, NL * NCOL:NL * NCOL + 8] = _col(inp["final_g"])
    wsT = np.ascontiguousarray(np.asarray(inp["w_s"], np.float32)[:NL].transpose(0, 3, 1, 2)).reshape(NL, 128, 512)
    cst = np.zeros((128, NCST), np.float32)
    ii = np.arange(128)
    cst[:, K_ID:K_ID + 128] = np.eye(128, dtype=np.float32)
    cst[:, K_MLE:K_MLE + 128] = (ii[:, None] <= ii[None, :]).astype(np.float32)
    cst[:, K_MBIG:K_MBIG + 128] = np.where(ii[:, None] <= ii[None, :], 0.0, 1e30).astype(np.float32)
    cst[:, K_BLK:K_BLK + 128] = ((ii[:, None] // 64) == (ii[None, :] // 64)).astype(np.float32) / 64.0
    for hh in range(4):
        cst[hh, K_SEL + hh * 128:K_SEL + (hh + 1) * 128] = 1.0
    cst[:, K_ONE:K_ONE + 128] = 1.0
    return dict(wflat=wflat, colp=colp, rowp=rowp, wsT=wsT, cst=cst)


_CACHE = {}


def run(inp, NB, S, NL, ncores):
    key = (NB, S, NL, False)
    if key not in _CACHE:
        _CACHE[key] = build(NB, S, NL)[0]
    nc = _CACHE[key]
    shared = host_layout(inp, NL)
    x = np.asarray(inp["x"], np.float32)
    mem = np.asarray(inp["mem"], np.float32)
    in_maps = []
    for c in range(ncores):
        m = dict(shared)
        m["x"] = np.ascontiguousarray(x[c * NB:(c + 1) * NB])
        m["mem"] = np.ascontiguousarray(mem[c * NB:(c + 1) * NB])
        in_maps.append(m)
    res = run_bass_kernel_spmd(nc, in_maps, core_ids=list(range(ncores)))
    return np.concatenate([np.asarray(r["out"]) for r in res.results], axis=0)


def run_chunked(inp, SCH, NL, ncores):
    key = (1, SCH, NL, True)
    if key not in _CACHE:
        _CACHE[key] = build(1, SCH, NL, stateio=True)[0]
    nc = _CACHE[key]
    shared = host_layout(inp, NL)
    x = np.asarray(inp["x"], np.float32)
    mem = np.asarray(inp["mem"], np.float32)
    B, S = x.shape[0], x.shape[1]
    out = np.zeros((B, S, D), np.float32)
    for g in range(0, B, ncores):
        nb = min(ncores, B - g)
        state = [np.zeros((128, NL * SL), np.float32) for _ in range(nb)]
        for ch in range(S // SCH):
            in_maps = []
            for c in range(nb):
                m = dict(shared)
                m["x"] = np.ascontiguousarray(x[g + c:g + c + 1, ch * SCH:(ch + 1) * SCH])
                m["mem"] = np.ascontiguousarray(mem[g + c:g + c + 1])
                m["st_in"] = state[c]
                in_maps.append(m)
            res = run_bass_kernel_spmd(nc, in_maps, core_ids=list(range(nb)))
            for c in range(nb):
                out[g + c, ch * SCH:(ch + 1) * SCH] = np.asarray(res.results[c]["out"])[0]
                state[c] = np.ascontiguousarray(np.asarray(res.results[c]["st_out"], np.float32))
    return out


def kernel(**inputs):
    inp = {k: np.asarray(v) for k, v in inputs.items()}
    return run(inp, 2, 4096, 2, 8).astype(np.float32)
```

```python
import numpy as np
import concourse.bass as bass
import concourse.mybir as mybir
from concourse.bass_utils import run_bass_kernel_spmd

F32 = mybir.dt.float32
BF16 = mybir.dt.bfloat16
ALU = mybir.AluOpType
AF = mybir.ActivationFunctionType
AX = mybir.AxisListType

D = 1024
NMEM = 256
DFF = 2816
T = 512
NJ = 4
NSLOT = 6
PIECE = 4096
PPL = 35
RMS_EPS = 1e-6
LN_EPS = 1e-5

C_NMIX, C_NX, C_NFFN = 0, 8, 16
C_CBW, C_CBB, C_GNG, C_GNB = 24, 86, 88, 90
C_CCW, C_CCB, C_SKP = 92, 108, 112
C_CFW, C_CFB = 116, 182
C_BIG, C_BFG = 204, 205
NCOL = 208
R_LNG, R_LNB, R_BS, R_HNG, R_MEMG = 0, 256, 512, 1024, 1536
NROW = 2560
K_ID, K_MLE, K_MBIG, K_BLK, K_SEL, K_ONE = 0, 128, 256, 384, 512, 1024
NCST = 1152


def _es(dt):
    return 2 if dt == BF16 else 4


class Sched:
    ENGS = ("pe", "act", "dve", "pool", "sp")

    def __init__(self, nc, ndsem=16):
        self.nc = nc
        self.q = {e: [] for e in self.ENGS}
        self.sem = {e: nc.alloc_semaphore("sem_" + e) for e in self.ENGS}
        self.cnt = {e: 0 for e in self.ENGS}
        self.dsem = [nc.alloc_semaphore("dsem%d" % i) for i in range(ndsem)]
        self.dn = 0
        self.seen = {e: {} for e in self.ENGS}
        self.rec = {}
        self.nwait = 0
        self.phase = ""
        self.labels = {e: [] for e in self.ENGS}

    def _iv(self, ap):
        dims = ap.ap
        off = int(ap.offset)
        es = _es(ap.dtype)
        sp = str(ap.space).upper()
        if "PSUM" in sp:
            return ap.tensor.name, 0, 2048
        if "DRAM" in sp:
            lo = off
            free = dims
        else:
            ps = dims[0][0]
            lo = off % ps if ps > 0 else off
            free = dims[1:]
        ext = 1
        for s, c in free:
            ext += (c - 1) * abs(s)
        return ap.tensor.name, lo * es, (lo + ext) * es

    def _deps(self, reads, writes):
        deps = {}
        for ap in reads:
            name, lo, hi = self._iv(ap)
            R = self.rec.get(name)
            if R:
                for (l, h, t) in R["w"]:
                    if l < hi and lo < h:
                        deps[t[0]] = max(deps.get(t[0], 0), t[1])
        for ap in writes:
            name, lo, hi = self._iv(ap)
            R = self.rec.get(name)
            if R:
                for (l, h, t) in R["w"]:
                    if l < hi and lo < h:
                        deps[t[0]] = max(deps.get(t[0], 0), t[1])
                for (e, l, h), t in R["r"].items():
                    if l < hi and lo < h:
                        deps[t[0]] = max(deps.get(t[0], 0), t[1])
        return deps

    def _commit(self, reads, writes, ticket):
        for ap in reads:
            name, lo, hi = self._iv(ap)
            R = self.rec.setdefault(name, {"w": [], "r": {}})
            R["r"][(ticket[0], lo, hi)] = ticket
        for ap in writes:
            name, lo, hi = self._iv(ap)
            R = self.rec.setdefault(name, {"w": [], "r": {}})
            R["w"] = [(l, h, t) for (l, h, t) in R["w"] if not (lo <= l and h <= hi)]
            R["r"] = {k: t for k, t in R["r"].items() if not (lo <= k[1] and k[2] <= hi)}
            R["w"].append((lo, hi, ticket))

    def op(self, eng, fn, reads, writes):
        deps = self._deps(reads, writes)
        waits = []
        for k, v in deps.items():
            if k == "pe" and eng == "pe":
                continue
            if self.seen[eng].get(k, 0) < v:
                self.seen[eng][k] = v
                waits.append((k, v))
        self.cnt[eng] += 1
        ticket = (eng, self.cnt[eng])
        self.q[eng].append((waits, fn, ticket))
        self.labels[eng].append((self.phase, tuple(waits)))
        self.nwait += len(waits)
        self._commit(reads, writes, ticket)

    def dma(self, out, in_, eng="sp"):
        k = self.dn % len(self.dsem)
        use = self.dn // len(self.dsem)
        self.dn += 1
        key = ("d", k)
        deps = self._deps([in_], [out])
        if use > 0:
            deps[key] = max(deps.get(key, 0), 16 * use)
        waits = []
        for kk, v in deps.items():
            if self.seen[eng].get(kk, 0) < v:
                self.seen[eng][kk] = v
                waits.append((kk, v))
        ticket = (key, 16 * (use + 1))
        self.q[eng].append((waits, lambda e: e.dma_start(out=out, in_=in_), ticket))
        self._commit([in_], [out], ticket)

    def _semof(self, k):
        return self.dsem[k[1]] if isinstance(k, tuple) else self.sem[k]

    def finish(self):
        waits = []
        n = len(self.dsem)
        for k in range(n):
            uses = (self.dn - k + n - 1) // n if self.dn > k else 0
            if uses > 0:
                waits.append((("d", k), 16 * uses))
        self.q["sp"].append((waits, None, None))

    def emit(self):
        nc = self.nc
        S = self

        def replay(name):
            def f(eng):
                for waits, fn, ticket in S.q[name]:
                    for k, v in waits:
                        eng.wait_ge(S._semof(k), v)
                    if fn is None:
                        continue
                    ins = fn(eng)
                    if isinstance(ticket[0], tuple):
                        ins.then_inc(S._semof(ticket[0]), 16)
                    else:
                        ins.then_inc(S.sem[ticket[0]], 1)
            return f

        with nc.Block() as block:
            block.tensor(replay("pe"))
            block.scalar(replay("act"))
            block.vector(replay("dve"))
            block.gpsimd(replay("pool"))
            block.sync(replay("sp"))

    def mm(self, out, lhsT, rhs, start=True, stop=True):
        self.op("pe", lambda e: e.matmul(out, lhsT, rhs, start=start, stop=stop), [lhsT, rhs], [out])

    def tr(self, out, in_, ident):
        self.op("pe", lambda e: e.transpose(out, in_, ident), [in_, ident], [out])

    def act(self, out, in_, func, bias=None, scale=None, accum_out=None):
        reads = [in_]
        writes = [out]
        kw = {}
        if bias is not None:
            kw["bias"] = bias
            if not isinstance(bias, (int, float)):
                reads.append(bias)
        if scale is not None:
            kw["scale"] = scale
            if not isinstance(scale, (int, float)):
                reads.append(scale)
        if accum_out is not None:
            kw["accum_out"] = accum_out
            writes.append(accum_out)
        self.op("act", lambda e: e.activation(out, in_, func, **kw), reads, writes)

    def tt(self, out, in0, in1, op, eng="dve"):
        self.op(eng, lambda e: e.tensor_tensor(out, in0, in1, op), [in0, in1], [out])

    def ts(self, out, in0, s1, s2, op0, op1=None, eng="dve"):
        reads = [in0]
        for s in (s1, s2):
            if s is not None and not isinstance(s, (int, float)):
                reads.append(s)
        if op1 is None:
            self.op(eng, lambda e: e.tensor_scalar(out, in0, s1, None, op0), reads, [out])
        else:
            self.op(eng, lambda e: e.tensor_scalar(out, in0, s1, s2, op0, op1), reads, [out])

    def stt(self, out, in0, scalar, in1, op0, op1):
        reads = [in0, in1]
        if not isinstance(scalar, (int, float)):
            reads.append(scalar)
        self.op("dve", lambda e: e.scalar_tensor_tensor(out, in0, scalar, in1, op0, op1), reads, [out])

    def copy(self, out, in_, eng="dve"):
        if eng == "act":
            self.act(out, in_, AF.Copy)
        else:
            self.op(eng, lambda e: e.tensor_copy(out, in_), [in_], [out])

    def memset(self, out, val, eng="pool"):
        self.op(eng, lambda e: e.memset(out, val), [], [out])


SL = 638


def build(NB, S, NL, stateio=False):
    nc = bass.Bass("TRN2", target_bir_lowering=False)
    NT = S // T
    NP = NL * PPL
    x = nc.dram_tensor("x", [NB, S, D], F32, kind="ExternalInput")
    mem = nc.dram_tensor("mem", [NB, NMEM, D], F32, kind="ExternalInput")
    wflat = nc.dram_tensor("wflat", [NP, 128, PIECE], F32, kind="ExternalInput")
    colp = nc.dram_tensor("colp", [128, NL * NCOL + 8], F32, kind="ExternalInput")
    rowp = nc.dram_tensor("rowp", [NL, NROW], F32, kind="ExternalInput")
    wsT = nc.dram_tensor("wsT", [NL, 128, 512], F32, kind="ExternalInput")
    cst = nc.dram_tensor("cst", [128, NCST], F32, kind="ExternalInput")
    out = nc.dram_tensor("out", [NB, S, D], F32, kind="ExternalOutput")
    wbf = nc.dram_tensor("wbf", [NP, 128, PIECE], BF16)
    if stateio:
        st_in = nc.dram_tensor("st_in", [128, NL * SL], F32, kind="ExternalInput")
        st_out = nc.dram_tensor("st_out", [128, NL * SL], F32, kind="ExternalOutput")

    A = nc.alloc_sbuf_tensor
    cst_t = A("cst_t", [128, NCST], F32)
    identb = A("identb", [128, 128], BF16)
    onesb = A("onesb", [128, 128], BF16)
    blkb = A("blkb", [128, 128], BF16)
    neghalf = A("neghalf", [128, 512], F32)
    epsr = A("epsr", [128, 2], F32)
    colp_t = A("colp_t", [128, NL * NCOL + 8], F32)
    nbfg = A("nbfg", [4, NL], F32)
    lnab = A("lnab", [128, NL, 512], F32)
    bsb = A("bsb", [128, NL, 2, 128], F32)
    hngb = A("hngb", [128, NL, 512], F32)
    wsTb = A("wsTb", [128, NL, 512], BF16)
    h = A("h", [128, 8, T], F32)
    hn = A("hn", [128, 8, T], BF16)
    ring = A("ring", [128, NSLOT, PIECE], BF16)
    kTx = A("kTx", [128, NL, 8, NMEM], BF16)
    Vx = A("Vx", [128, NL, 2, D], BF16)
    Cst = A("Cst", [128, NL, 4, 128], F32)
    Cbf = A("Cbf", [128, NL, 4, 128], BF16)
    nst = A("nst", [128, NL, 4], F32)
    nbf = A("nbf", [128, NL, 4], BF16)
    yhist = A("yhist", [128, NL, 2, 32 + T], BF16)
    xcb = A("xcb", [128, NL, 4, 4 + T], BF16)
    fhist = A("fhist", [128, NL, 22, 2], F32)
    Brow = A("Brow", [4, NL, 129], F32)
    Mrow = A("Mrow", [4, NL, 129], F32)
    xin = A("xin", [128, 2, D], F32)
    ostage = A("ostage", [128, 2, D], F32)
    rstd = A("rstd", [128, T], F32)
    t0 = A("t0", [128, T], F32)
    sqn = A("sqn", [128, 2, T], BF16)
    small = A("small", [128, 64], F32)
    stio = A("stio", [128, NL * SL], F32)
    NA = 12960
    arena = A("arena", [128, NA], F32)
    psb = [nc.alloc_psum_tensor("ps%d" % i, [128, 512], F32) for i in range(8)]

    S_ = Sched(nc)
    st = {"ps": 0}

    def psum():
        p = psb[st["ps"] % 8]
        st["ps"] += 1
        return p

    class Carver:
        def __init__(self):
            self.o = 0

        def f32(self, *shape):
            n = int(np.prod(shape))
            v = arena[:, self.o:self.o + n]
            self.o += n
            assert self.o <= NA, self.o
            return self._shape(v, shape)

        def bf(self, *shape):
            n = int(np.prod(shape))
            n32 = (n + 1) // 2
            v = arena[:, self.o:self.o + n32].bitcast(BF16)[:, 0:n]
            self.o += n32
            assert self.o <= NA, self.o
            return self._shape(v, shape)

        @staticmethod
        def _shape(v, shape):
            if len(shape) == 1:
                return v
            if len(shape) == 2:
                return v.rearrange("p (a b) -> p a b", b=shape[1])
            return v.rearrange("p (a b c) -> p a b c", b=shape[1], c=shape[2])

    ident = cst_t[:, K_ID:K_ID + 128]
    mle = cst_t[:, K_MLE:K_MLE + 128]
    mbig = cst_t[:, K_MBIG:K_MBIG + 128]
    onesf = cst_t[:, K_ONE:K_ONE + 128]

    def colL(l, c, n=1, p0=0, p1=128):
        return colp_t[p0:p1, l * NCOL + c:l * NCOL + c + n]

    S_.dma(cst_t[:, :], cst[:, :])
    S_.dma(colp_t[:, :], colp[:, :])
    S_.copy(identb[:, :], ident)
    S_.memset(onesb[:, :], 1.0)
    S_.copy(blkb[:, :], cst_t[:, K_BLK:K_BLK + 128])
    S_.memset(neghalf[:, :], -0.5)
    S_.memset(epsr[:, 0:1], RMS_EPS)
    S_.memset(epsr[:, 1:2], LN_EPS)
    cv = Carver()
    wtmp = cv.f32(512)
    for l in range(NL):
        S_.dma(lnab[:, l, :], rowp[l, R_LNG:R_LNG + 512].partition_broadcast(128))
        S_.dma(hngb[:, l, :], rowp[l, R_HNG:R_HNG + 512].partition_broadcast(128))
        for ncn in range(2):
            for hf in range(2):
                hh = 2 * ncn + hf
                S_.dma(bsb[hf * 64:(hf + 1) * 64, l, ncn, :],
                       rowp[l, R_BS + hh * 128:R_BS + (hh + 1) * 128].partition_broadcast(64))
        S_.dma(wtmp, wsT[l, :, :])
        S_.tt(wsTb[:, l, :].rearrange("p (a b) -> p a b", b=128),
              wtmp.rearrange("p (a b) -> p a b", b=128),
              mle.unsqueeze(1).to_broadcast([128, 4, 128]), ALU.mult)
        S_.ts(nbfg[:, l:l + 1], colL(l, C_BFG, 1, 0, 4), -1.0, None, ALU.mult)

    cv = Carver()
    stage = [cv.f32(PIECE) for _ in range(3)]
    hb = h[:, :, :].rearrange("p a b -> p (a b)").bitcast(BF16)
    obf = [hb[:, 0:PIECE], hb[:, PIECE:2 * PIECE], hn[:, :, :].rearrange("p a b -> p (a b)")]
    cast_eng = ["dve", "pool", "act"]
    for i in range(NP):
        S_.dma(stage[i % 3], wflat[i, :, :])
        S_.copy(obf[i % 3], stage[i % 3], eng=cast_eng[i % 3])
        S_.dma(wbf[i, :, :], obf[i % 3])

    order = []
    for b in range(NB):
        for l in range(NL):
            order += [l * PPL + k for k in range(4)]
        for ti in range(NT):
            for l in range(NL):
                order += [l * PPL + k for k in range(4, PPL)]
    ws = {"issued": 0, "pos": 0}

    def wnext(expect):
        while ws["issued"] < min(len(order), ws["pos"] + NSLOT):
            i = ws["issued"]
            S_.dma(ring[:, i % NSLOT, :], wbf[order[i], :, :])
            ws["issued"] += 1
        assert order[ws["pos"]] == expect, (order[ws["pos"]], expect)
        slot = ws["pos"] % NSLOT
        ws["pos"] += 1
        return ring[:, slot, :]

    def w3(v, n):
        return v.rearrange("p (a b) -> p a b", b=n)

    def rms_stats(src):
        ps = psum()
        for c in range(8):
            S_.act(sqn[:, c % 2, :], src[:, c, :], AF.Square)
            S_.mm(ps[:, :], onesb[:, :], sqn[:, c % 2, :], start=(c == 0), stop=(c == 7))
        S_.act(t0[:, :], ps[:, :], AF.Ln, bias=epsr[:, 0:1], scale=1.0 / D)
        S_.act(rstd[:, :], t0[:, :], AF.Exp, scale=-0.5)

    def norm_to_hn(gcol):
        rms_stats(h)
        for c in range(8):
            S_.stt(hn[:, c, :], h[:, c, :], gcol[:, c:c + 1], rstd[:, :], ALU.mult, ALU.mult)

    def proj_fm(W, col0, rhs_t, nk, ps_ap):
        for kc in range(nk):
            S_.mm(ps_ap, W[:, kc, col0:col0 + 128], rhs_t[:, kc, :], start=(kc == 0), stop=(kc == nk - 1))

    def resid_proj(pid0, src):
        for half in range(2):
            W = w3(wnext(pid0 + half), 512)
            for n4 in range(4):
                ncn = half * 4 + n4
                ps = psum()
                proj_fm(W, n4 * 128, src, 8, ps[:, :])
                S_.tt(h[:, ncn, :], h[:, ncn, :], ps[:, :], ALU.add)

    def rsqrt_small(out, in_, eps, n):
        S_.ts(out, in_, eps, None, ALU.add)
        S_.tt(out, out, neghalf[0:out.shape[0], 0:n], ALU.pow, eng="pool")

    for b in range(NB):
        for l in range(NL):
            cv = Carver()
            memt = cv.f32(2, D)
            memg = cv.f32(D)
            mnb = cv.bf(2, D)
            mnT = cv.bf(8, NMEM)
            junk = cv.f32(D)
            S_.dma(memg, rowp[l, R_MEMG:R_MEMG + D].partition_broadcast(128))
            ss = small[:, 0:2]
            rs2 = small[:, 2:4]
            for mc in range(2):
                S_.dma(memt[:, mc, :], mem[b, mc * 128:(mc + 1) * 128, :])
                S_.act(junk, memt[:, mc, :], AF.Square, accum_out=ss[:, mc:mc + 1])
            S_.ts(rs2, ss, 1.0 / D, RMS_EPS, ALU.mult, ALU.add)
            S_.tt(rs2, rs2, neghalf[:, 0:2], ALU.pow, eng="pool")
            for mc in range(2):
                S_.stt(mnb[:, mc, :], memt[:, mc, :], rs2[:, mc:mc + 1], memg, ALU.mult, ALU.mult)
                for half in range(2):
                    ps = psum()
                    pv = ps[:, :].bitcast(BF16)
                    for k4 in range(4):
                        kc = half * 4 + k4
                        S_.tr(pv[:, k4 * 128:(k4 + 1) * 128], mnb[:, mc, kc * 128:(kc + 1) * 128], identb[:, :])
                    S_.copy(mnT[:, half * 4:half * 4 + 4, mc * 128:(mc + 1) * 128],
                            pv[:, 0:512].rearrange("p (a b) -> p a b", b=128), eng="act")
            for pk in range(2):
                W = w3(wnext(l * PPL + pk), 512)
                for d4 in range(4):
                    dch = pk * 4 + d4
                    ps = psum()
                    proj_fm(W, d4 * 128, mnT, 8, ps[:, 0:NMEM])
                    S_.copy(kTx[:, l, dch, :], ps[:, 0:NMEM], eng="act")
            for pv_ in range(2):
                W = w3(wnext(l * PPL + 2 + pv_), 512)
                for mc in range(2):
                    ps = psum()
                    for kc in range(8):
                        S_.mm(ps[:, :], mnT[:, kc, mc * 128:(mc + 1) * 128], W[:, kc, :], start=(kc == 0), stop=(kc == 7))
                    S_.copy(Vx[:, l, mc, pv_ * 512:(pv_ + 1) * 512], ps[:, :], eng="act")
        S_.memset(Cst[:, :, :, :], 0.0)
        S_.memset(Cbf[:, :, :, :], 0.0)
        S_.memset(nst[:, :, :], 0.0)
        S_.memset(nbf[:, :, :], 0.0)
        S_.memset(yhist[:, :, :, 0:32], 0.0)
        S_.memset(xcb[:, :, :, 0:4], 0.0)
        S_.memset(fhist[:, :, :, :], 0.0)
        S_.memset(Brow[:, :, 0:1], 0.0)
        S_.memset(Mrow[:, :, 0:1], 0.0)
        if stateio:
            S_.dma(stio[:, :], st_in[:, :])
            for l in range(NL):
                o = l * SL
                S_.copy(Cst[:, l, :, :], stio[:, o:o + 512].rearrange("p (a b) -> p a b", b=128))
                S_.copy(nst[:, l, :], stio[:, o + 512:o + 516])
                S_.copy(yhist[:, l, :, 2:32], stio[:, o + 516:o + 576].rearrange("p (a b) -> p a b", b=30))
                S_.copy(xcb[:, l, :, 0:4], stio[:, o + 576:o + 592].rearrange("p (a b) -> p a b", b=4))
                S_.copy(fhist[:, l, :, :], stio[:, o + 592:o + 636].rearrange("p (a b) -> p a b", b=2))
                S_.copy(Brow[:, l, 0:1], stio[0:4, o + 636:o + 637])
                S_.copy(Mrow[:, l, 0:1], stio[0:4, o + 637:o + 638])
                S_.copy(Cbf[:, l, :, :], Cst[:, l, :, :], eng="pool")
                S_.copy(nbf[:, l, :], nst[:, l, :], eng="pool")

        for ti in range(NT):
            tok0 = ti * T
            S_.phase = "xload"
            for j in range(NJ):
                S_.dma(xin[:, j % 2, :], x[b, tok0 + j * 128:tok0 + (j + 1) * 128, :])
                for half in range(2):
                    ps = psum()
                    for c4 in range(4):
                        c = half * 4 + c4
                        S_.tr(ps[:, c4 * 128:(c4 + 1) * 128], xin[:, j % 2, c * 128:(c + 1) * 128], ident)
                    S_.copy(h[:, half * 4:half * 4 + 4, j * 128:(j + 1) * 128],
                            ps[:, :].rearrange("p (a b) -> p a b", b=128), eng="act")

            for l in range(NL):
                pid = l * PPL
                S_.phase = "mixnorm"
                norm_to_hn(colL(l, C_NMIX, 8))
                cv = Carver()
                ycat = cv.bf(8, T)
                xconv = cv.bf(4, T)
                sgo = cv.bf(4, T)
                logi_row = cv.f32(T)
                sp_row = cv.f32(T)
                mark = cv.o
                S_.phase = "A"
                u = cv.bf(2, T)
                vn = cv.bf(4, 256)
                vg = [cv.f32(256) for _ in range(NJ)]
                W = w3(wnext(pid + 4), 512)
                for ncn in range(2):
                    ps = psum()
                    proj_fm(W, ncn * 128, hn, 8, ps[:, :])
                    S_.act(u[:, ncn, :], ps[:, :], AF.Gelu)
                for j in range(NJ):
                    js = slice(j * 128, (j + 1) * 128)
                    ps = psum()
                    for kc in range(8):
                        S_.mm(ps[:, 0:256], hn[:, kc, js], W[:, kc, 256:512], start=(kc == 0), stop=(kc == 7))
                    S_.act(vg[j], ps[:, 0:256], AF.Gelu)
                for j in range(NJ):
                    v_ = vg[j]
                    so = 8 + (j % 2) * 12
                    st6 = small[:, so:so + 6]
                    mv = small[:, so + 6:so + 8]
                    rsa = small[:, so + 8:so + 9]
                    S_.op("dve", lambda e, o=st6, i=v_: e.bn_stats(o, i), [v_], [st6])
                    S_.op("dve", lambda e, o=mv, i=st6: e.bn_aggr(o, i), [st6], [mv])
                    rsqrt_small(rsa, mv[:, 1:2], LN_EPS, 1)
                    S_.ts(v_, v_, mv[:, 0:1], rsa, ALU.subtract, ALU.mult)
                    S_.tt(v_, v_, lnab[:, l, 0:256], ALU.mult)
                    S_.tt(vn[:, j, :], v_, lnab[:, l, 256:512], ALU.add)
                cv.o = mark
                colsJ = [cv.f32(20) for _ in range(NJ)]
                wTJ = [cv.f32(4, 128) for _ in range(NJ)]
                r2 = cv.o
                a_R = cv.f32(4, 128)
                rtmp = cv.f32(128)
                arg = cv.f32(4, 128)
                r3 = cv.o
                cv.o = r2
                sgB = cv.f32(T)
                tmpa = cv.f32(2, 128)
                cv.o = r3
                y2 = cv.f32(T)
                y2b = cv.bf(T)
                dB = cv.f32(T)
                sqd = cv.bf(T)
                t1 = cv.f32(T)
                tmpB = cv.f32(T)
                diag = cv.bf(31, 128)
                cv.o = r3
                cvt = cv.f32(T)
                sgc = cv.f32(T)
                cv.o = r3
                qTb = cv.bf(4, 128)
                kTb = cv.bf(4, 128)
                kwb = cv.bf(4, 128)
                vtb = cv.bf(4, 128)
                scT = cv.bf(4, 128)
                n1 = cv.f32(4, 128)
                hs = cv.f32(4, 128)
                hnt = cv.f32(4, 128)
                t2 = n1
                S_.phase = "B"
                W = w3(wnext(pid + 5), 512)
                for c in range(2):
                    psa = psum()
                    proj_fm(W, c * 128, hn, 8, psa[:, :])
                    psg = psum()
                    proj_fm(W, 256 + c * 128, hn, 8, psg[:, :])
                    S_.act(sgB, psg[:, :], AF.Sigmoid)
                    S_.tt(yhist[:, l, c, 32:32 + T], psa[:, :], sgB, ALU.mult)
                S_.phase = "Cproj"
                W = w3(wnext(pid + 6), 512)
                for hh in range(4):
                    ps = psum()
                    proj_fm(W, hh * 128, hn, 8, ps[:, :])
                    S_.copy(xcb[:, l, hh, 4:4 + T], ps[:, :], eng="act")
                W = w3(wnext(pid + 7), 512)
                for hh in range(4):
                    ps = psum()
                    proj_fm(W, hh * 128, hn, 8, ps[:, :])
                    S_.act(sgo[:, hh, :], ps[:, :], AF.Sigmoid)
                W = w3(wnext(pid + 8)[:, 0:64], 8)
                psi = psum()
                for kc in range(8):
                    S_.mm(psi[0:4, :], W[:, kc, 0:4], hn[:, kc, :], start=(kc == 0), stop=(kc == 7))
                psf = psum()
                for kc in range(8):
                    S_.mm(psf[0:4, :], W[:, kc, 4:8], hn[:, kc, :], start=(kc == 0), stop=(kc == 7))
                for hh in range(4):
                    S_.ts(cvt, xcb[:, l, hh, 1:1 + T], colL(l, C_CCW + hh), colL(l, C_CCB + hh), ALU.mult, ALU.add)
                    for jt in range(1, 4):
                        S_.stt(cvt, xcb[:, l, hh, 1 + jt:1 + jt + T], colL(l, C_CCW + jt * 4 + hh), cvt, ALU.mult, ALU.add)
                    S_.act(sgc, cvt, AF.Sigmoid)
                    S_.tt(xconv[:, hh, :], cvt, sgc, ALU.mult)
                S_.phase = "A"
                for j in range(NJ):
                    js = slice(j * 128, (j + 1) * 128)
                    ps = psum()
                    for hh in range(4):
                        p0 = (hh % 2) * 64
                        S_.mm(ps[p0:p0 + 64, (hh // 2) * 128:(hh // 2 + 1) * 128],
                              vn[:, j, hh * 64:(hh + 1) * 64], wsTb[:, l, hh * 128:(hh + 1) * 128])
                    S_.tt(tmpa, ps[:, 0:256].rearrange("p (a b) -> p a b", b=128), bsb[:, l, :, :], ALU.add)
                    S_.tt(ycat[:, 0:2, js], tmpa, u[:, 0:2, js], ALU.mult)
                S_.phase = "Cgate"
                S_.act(logi_row[0:4, :], psi[0:4, :], AF.Identity, bias=colL(l, C_BIG, 1, 0, 4))
                S_.act(sp_row[0:4, :], psf[0:4, :], AF.Exp, bias=nbfg[:, l:l + 1], scale=-1.0)
                S_.act(sp_row[0:4, :], sp_row[0:4, :], AF.Ln, bias=1.0)
                S_.phase = "B"
                for c in range(2):
                    for jt in range(31):
                        S_.act(diag[:, jt, :], identb[:, :], AF.Copy, scale=colL(l, C_CBW + jt * 2 + c))
                    psc31 = psum()
                    for jt in range(31):
                        S_.mm(psc31[:, :], diag[:, jt, :], yhist[:, l, c, 2 + jt:2 + jt + T], start=(jt == 0), stop=(jt == 30))
                    S_.copy(yhist[:, l, c, 0:32], yhist[:, l, c, T:T + 32], eng="pool")
                    S_.act(y2, psc31[:, :], AF.Identity, bias=colL(l, C_CBB + c))
                    S_.act(y2b, psc31[:, :], AF.Identity, bias=colL(l, C_CBB + c))
                    if c == 0:
                        S_.phase = "Cgate"
                        for j in range(NJ):
                            js = slice(j * 128, (j + 1) * 128)
                            cols = colsJ[j]
                            wT = wTJ[j]
                            Bn = Brow[:, l, 1:129]
                            Mn = Mrow[:, l, 1:129]
                            S_.op("dve", lambda e, o=Bn, d1=sp_row[0:4, js], i0=Brow[:, l, 0:1]:
                                  e.tensor_tensor_scan(o, onesf[0:4, :], d1, i0, ALU.mult, ALU.subtract),
                                  [onesf[0:4, :], sp_row[0:4, js], Brow[:, l, 0:1]], [Bn])
                            S_.tt(a_R[0:4, 0, :], logi_row[0:4, js], Bn, ALU.subtract)
                            S_.op("dve", lambda e, o=Mn, d1=a_R[0:4, 0, :], i0=Mrow[:, l, 0:1]:
                                  e.tensor_tensor_scan(o, onesf[0:4, :], d1, i0, ALU.mult, ALU.max),
                                  [onesf[0:4, :], a_R[0:4, 0, :], Mrow[:, l, 0:1]], [Mn])
                            S_.act(a_R[0:4, 1, :], Mn, AF.Exp, bias=Mrow[:, l, 0:1], scale=-1.0)
                            S_.tt(rtmp[0:4, :], Bn, Mn, ALU.add)
                            S_.act(a_R[0:4, 2, :], rtmp[0:4, :], AF.Exp, scale=-1.0)
                            negM = small[0:4, 20:21]
                            S_.ts(negM, Mrow[:, l, 128:129], -1.0, None, ALU.mult)
                            S_.act(a_R[0:4, 3, :], a_R[0:4, 0, :], AF.Exp, bias=negM)
                            dg = small[0:4, 24:28]
                            S_.ts(dg, ident[0:4, 0:4], a_R[0:4, 1, 127:128], None, ALU.mult)
                            psc = psum()
                            for q in range(4):
                                S_.tr(psc[:, q * 4:(q + 1) * 4], a_R[0:4, q, :], ident[0:4, 0:4])
                            S_.mm(psc[:, 16:20], onesf[0:4, :], dg)
                            S_.copy(cols, psc[:, 0:20])
                            psM = psum()
                            for hh in range(4):
                                S_.mm(psM[:, hh * 128:(hh + 1) * 128], cst_t[0:4, K_SEL + hh * 128:K_SEL + (hh + 1) * 128], Mn)
                            S_.tt(arg, psM[:, :].rearrange("p (a b) -> p a b", b=128),
                                  mbig.unsqueeze(1).to_broadcast([128, 4, 128]), ALU.add)
                            S_.copy(Brow[:, l, 0:1], Brow[:, l, 128:129], eng="pool")
                            S_.copy(Mrow[:, l, 0:1], Mrow[:, l, 128:129], eng="pool")
                            for hh in range(4):
                                S_.act(wT[:, hh, :], arg[:, hh, :], AF.Exp, bias=cols[:, hh:hh + 1], scale=-1.0)
                        S_.phase = "B"
                    psm = psum()
                    S_.mm(psm[:, :], blkb[:, :], y2b)
                    S_.tt(dB, y2, psm[:, :], ALU.subtract)
                    S_.act(sqd, dB, AF.Square)
                    psv = psum()
                    S_.mm(psv[:, :], blkb[:, :], sqd)
                    S_.act(t1, psv[:, :], AF.Ln, bias=epsr[:, 1:2])
                    S_.act(t1, t1, AF.Exp, scale=-0.5)
                    S_.stt(tmpB, dB, colL(l, C_GNG + c), t1, ALU.mult, ALU.mult)
                    S_.act(y2, tmpB, AF.Sigmoid, bias=colL(l, C_GNB + c))
                    S_.stt(ycat[:, 2 + c, :], tmpB, colL(l, C_GNB + c), y2, ALU.add, ALU.mult)
                Wq = wnext(pid + 9)[:, 0:1536].rearrange("p (a b c) -> p a b c", b=4, c=128)
                S_.phase = "Cloop"
                for j in range(NJ):
                    js = slice(j * 128, (j + 1) * 128)
                    psq = psum()
                    for hh in range(4):
                        S_.mm(psq[:, hh * 128:(hh + 1) * 128], Wq[:, 0, hh, :], xconv[:, hh, js])
                    S_.act(qTb, psq[:, :].rearrange("p (a b) -> p a b", b=128), AF.Copy, scale=128.0 ** -0.5)
                    psk = psum()
                    for hh in range(4):
                        S_.mm(psk[:, hh * 128:(hh + 1) * 128], Wq[:, 1, hh, :], xconv[:, hh, js])
                    S_.copy(kTb, psk[:, :].rearrange("p (a b) -> p a b", b=128), eng="act")
                    pskt = psum()
                    for hh in range(4):
                        S_.mm(pskt[:, hh * 128:(hh + 1) * 128], xconv[:, hh, js], Wq[:, 1, hh, :])
                    psvt = psum()
                    for hh in range(4):
                        S_.mm(psvt[:, hh * 128:(hh + 1) * 128], xcb[:, l, hh, 4 + j * 128:4 + (j + 1) * 128], Wq[:, 2, hh, :])
                    S_.copy(vtb, psvt[:, :].rearrange("p (a b) -> p a b", b=128), eng="act")
                    pss = psum()
                    for hh in range(4):
                        S_.mm(pss[:, hh * 128:(hh + 1) * 128], kTb[:, hh, :], qTb[:, hh, :])
                    psP1 = psum()
                    psD = psum()
                    for hh in range(4):
                        S_.mm(psP1[:, hh * 128:(hh + 1) * 128], qTb[:, hh, :], Cbf[:, l, hh, :])
                        S_.mm(psD[:, hh:hh + 1], qTb[:, hh, :], nbf[:, l, hh:hh + 1])
                    cols = colsJ[j]
                    wT = wTJ[j]
                    S_.tt(kwb, pskt[:, :].rearrange("p (a b) -> p a b", b=128),
                          cols[:, 12:16].unsqueeze(2).to_broadcast([128, 4, 128]), ALU.mult)
                    S_.tt(scT, pss[:, :].rearrange("p (a b) -> p a b", b=128), wT, ALU.mult)
                    psP2 = psum()
                    psD_b = psum()
                    for hh in range(4):
                        S_.mm(psP2[:, hh * 128:(hh + 1) * 128], scT[:, hh, :], vtb[:, hh, :])
                        S_.mm(psD_b[:, hh:hh + 1], scT[:, hh, :], onesb[:, 0:1])
                    den = small[:, 28:32]
                    rden = small[:, 32:36]
                    S_.tt(den, psD[:, 0:4], cols[:, 4:8], ALU.mult)
                    S_.tt(den, den, psD_b[:, 0:4], ALU.add)
                    S_.stt(den, den, -1.0, den, ALU.mult, ALU.max)
                    S_.tt(den, den, cols[:, 8:12], ALU.max)
                    S_.op("dve", lambda e, o=rden, i=den: e.reciprocal(o, i), [den], [rden])
                    for hh in range(4):
                        S_.act(n1[:, hh, :], psP1[:, hh * 128:(hh + 1) * 128], AF.Copy, scale=cols[:, 4 + hh:5 + hh])
                    S_.tt(hs, n1, psP2[:, :].rearrange("p (a b) -> p a b", b=128), ALU.add)
                    S_.tt(hs, hs, rden.unsqueeze(2).to_broadcast([128, 4, 128]), ALU.mult)
                    st24 = small[:, 36:60].rearrange("p (a b) -> p a b", b=6)
                    mv8 = small[:, 0:8].rearrange("p (a b) -> p a b", b=2)
                    for hh in range(4):
                        S_.op("dve", lambda e, o=st24[:, hh, :], i=hs[:, hh, :]: e.bn_stats(o, i), [hs[:, hh, :]], [st24[:, hh, :]])
                        S_.op("dve", lambda e, o=mv8[:, hh, :], i=st24[:, hh, :]: e.bn_aggr(o, i), [st24[:, hh, :]], [mv8[:, hh, :]])
                    rsc = small[:, 60:64]
                    S_.ts(rsc, mv8[:, :, 1], LN_EPS, None, ALU.add)
                    S_.tt(rsc, rsc, neghalf[:, 0:4], ALU.pow, eng="pool")
                    for hh in range(4):
                        S_.ts(hnt[:, hh, :], hs[:, hh, :], mv8[:, hh, 0:1], rsc[:, hh:hh + 1], ALU.subtract, ALU.mult)
                    S_.tt(hnt, hnt, hngb[:, l, :].rearrange("p (a b) -> p a b", b=128), ALU.mult)
                    psT = psum()
                    for hh in range(4):
                        S_.tr(psT[:, hh * 128:(hh + 1) * 128], hnt[:, hh, :], ident)
                    for hh in range(4):
                        S_.stt(t2[:, hh, :], xconv[:, hh, js], colL(l, C_SKP + hh), psT[:, hh * 128:(hh + 1) * 128], ALU.mult, ALU.add)
                    S_.tt(ycat[:, 4:8, js], t2, sgo[:, :, js], ALU.mult)
                    psU = psum()
                    psD2 = psum()
                    for hh in range(4):
                        S_.mm(psU[:, hh * 128:(hh + 1) * 128], kwb[:, hh, :], vtb[:, hh, :])
                        S_.mm(psD2[:, hh:hh + 1], kwb[:, hh, :], onesb[:, 0:1])
                    for hh in range(4):
                        S_.stt(Cst[:, l, hh, :], Cst[:, l, hh, :], cols[:, 16 + hh:17 + hh], psU[:, hh * 128:(hh + 1) * 128], ALU.mult, ALU.add)
                    S_.tt(nst[:, l, :], nst[:, l, :], cols[:, 16:20], ALU.mult)
                    S_.tt(nst[:, l, :], nst[:, l, :], psD2[:, 0:4], ALU.add)
                    S_.copy(Cbf[:, l, :, :], Cst[:, l, :, :], eng="pool")
                    S_.copy(nbf[:, l, :], nst[:, l, :], eng="pool")
                for hh in range(4):
                    S_.copy(xcb[:, l, hh, 0:4], xcb[:, l, hh, T:T + 4], eng="pool")
                S_.phase = "outproj"
                resid_proj(pid + 10, ycat)

                S_.phase = "xattn"
                norm_to_hn(colL(l, C_NX, 8))
                cv = Carver()
                qx = cv.bf(8, T)
                oT = cv.bf(8, T)
                pT = cv.bf(8, T)
                pexp2 = [cv.bf(4, NMEM) for _ in range(2)]
                pn2 = [cv.bf(4, NMEM) for _ in range(2)]
                for half in range(2):
                    W = w3(wnext(pid + 12 + half), 512)
                    for n4 in range(4):
                        ps = psum()
                        proj_fm(W, n4 * 128, hn, 8, ps[:, :])
                        S_.copy(qx[:, half * 4 + n4, :], ps[:, :], eng="act")
                def qk_scores(j):
                    js = slice(j * 128, (j + 1) * 128)
                    pss2 = [psum(), psum()]
                    for hh in range(4):
                        pp = pss2[hh // 2]
                        o0 = (hh % 2) * NMEM
                        for dc in range(2):
                            S_.mm(pp[:, o0:o0 + NMEM], qx[:, hh * 2 + dc, js], kTx[:, l, hh * 2 + dc, :], start=(dc == 0), stop=(dc == 1))
                    return pss2

                pend = qk_scores(0)
                for j in range(NJ):
                    js = slice(j * 128, (j + 1) * 128)
                    so = (j % 2) * 16
                    mx = small[:, so + 0:so + 4]
                    nmx = small[:, so + 4:so + 8]
                    ssum = small[:, so + 8:so + 12]
                    rsx = small[:, so + 12:so + 16]
                    pexp = pexp2[j % 2]
                    pn = pn2[j % 2]
                    pss2 = pend
                    if j + 1 < NJ:
                        pend = qk_scores(j + 1)
                    for pr in range(2):
                        S_.op("dve", lambda e, o=mx[:, pr * 2:pr * 2 + 2], i=pss2[pr][:, :].rearrange("p (a b) -> p a b", b=NMEM):
                              e.tensor_reduce(o, i, AX.X, ALU.max),
                              [pss2[pr][:, :]], [mx[:, pr * 2:pr * 2 + 2]])
                    S_.ts(nmx, mx, -1.0 / 16.0, None, ALU.mult)
                    for hh in range(4):
                        pp = pss2[hh // 2]
                        o0 = (hh % 2) * NMEM
                        S_.act(pexp[:, hh, :], pp[:, o0:o0 + NMEM], AF.Exp, bias=nmx[:, hh:hh + 1], scale=1.0 / 16.0,
                               accum_out=ssum[:, hh:hh + 1])
                    S_.op("dve", lambda e, o=rsx, i=ssum: e.reciprocal(o, i), [ssum], [rsx])
                    S_.tt(pn, pexp, rsx.unsqueeze(2).to_broadcast([128, 4, NMEM]), ALU.mult)
                    ps = psum()
                    pv = ps[:, :].bitcast(BF16)
                    for hh in range(4):
                        for mc in range(2):
                            q_ = hh * 2 + mc
                            S_.tr(pv[:, q_ * 128:(q_ + 1) * 128], pn[:, hh, mc * 128:(mc + 1) * 128], identb[:, :])
                    S_.copy(pT[:, :, js], pv[:, :].rearrange("p (a b) -> p a b", b=128), eng="act")
                for hh in range(4):
                    for dc in range(2):
                        ps = psum()
                        for mc in range(2):
                            S_.mm(ps[:, :], Vx[:, l, mc, hh * 256 + dc * 128:hh * 256 + (dc + 1) * 128], pT[:, hh * 2 + mc, :],
                                  start=(mc == 0), stop=(mc == 1))
                        S_.copy(oT[:, hh * 2 + dc, :], ps[:, :], eng="act")
                resid_proj(pid + 14, oT)

                S_.phase = "ffn"
                norm_to_hn(colL(l, C_NFFN, 8))
                cv = Carver()
                actb = cv.bf(22, T)
                gx = [cv.f32(T + 2) for _ in range(2)]
                acc = [cv.f32(T) for _ in range(2)]
                gl = [cv.f32(T) for _ in range(2)]
                for p in range(11):
                    W = w3(wnext(pid + 16 + p), 512)
                    for cc in range(2):
                        c = 2 * p + cc
                        psg = psum()
                        proj_fm(W, cc * 128, hn, 8, psg[:, :])
                        psv = psum()
                        proj_fm(W, 256 + cc * 128, hn, 8, psv[:, :])
                        g_ = gx[c % 2]
                        a_ = acc[c % 2]
                        l_ = gl[c % 2]
                        S_.copy(g_[:, 0:2], fhist[:, l, c, :], eng="pool")
                        S_.copy(g_[:, 2:T + 2], psg[:, :], eng="act")
                        S_.copy(fhist[:, l, c, :], g_[:, T:T + 2], eng="pool")
                        S_.act(a_, psg[:, :], AF.Identity, bias=colL(l, C_CFB + c), scale=colL(l, C_CFW + 2 * 22 + c))
                        S_.stt(a_, g_[:, 1:T + 1], colL(l, C_CFW + 1 * 22 + c), a_, ALU.mult, ALU.add)
                        S_.stt(a_, g_[:, 0:T], colL(l, C_CFW + c), a_, ALU.mult, ALU.add)
                        S_.act(l_, a_, AF.Gelu)
                        S_.tt(actb[:, c, :], l_, psv[:, :], ALU.mult)
                for ncn in range(8):
                    W = w3(wnext(pid + 27 + ncn)[:, 0:22 * 128], 128)
                    ps = psum()
                    for kc in range(22):
                        S_.mm(ps[:, :], W[:, kc, :], actb[:, kc, :], start=(kc == 0), stop=(kc == 21))
                    S_.tt(h[:, ncn, :], h[:, ncn, :], ps[:, :], ALU.add)

            S_.phase = "final"
            rms_stats(h)
            cv = Carver()
            of = cv.f32(8, T)
            fg = colp_t[:, NL * NCOL:NL * NCOL + 8]
            for c in range(8):
                S_.stt(of[:, c, :], h[:, c, :], fg[:, c:c + 1], rstd[:, :], ALU.mult, ALU.mult)
            for j in range(NJ):
                js = slice(j * 128, (j + 1) * 128)
                for half in range(2):
                    ps = psum()
                    for c4 in range(4):
                        c = half * 4 + c4
                        S_.tr(ps[:, c4 * 128:(c4 + 1) * 128], of[:, c, js], ident)
                    S_.copy(ostage[:, j % 2, half * 512:(half + 1) * 512], ps[:, :], eng="act")
                S_.dma(out[b, tok0 + j * 128:tok0 + (j + 1) * 128, :], ostage[:, j % 2, :])

    if stateio:
        S_.memset(stio[:, :], 0.0)
        for l in range(NL):
            o = l * SL
            S_.copy(stio[:, o:o + 512].rearrange("p (a b) -> p a b", b=128), Cst[:, l, :, :])
            S_.copy(stio[:, o + 512:o + 516], nst[:, l, :])
            S_.copy(stio[:, o + 516:o + 576].rearrange("p (a b) -> p a b", b=30), yhist[:, l, :, 2:32])
            S_.copy(stio[:, o + 576:o + 592].rearrange("p (a b) -> p a b", b=4), xcb[:, l, :, 0:4])
            S_.copy(stio[:, o + 592:o + 636].rearrange("p (a b) -> p a b", b=2), fhist[:, l, :, :])
            S_.copy(stio[0:4, o + 636:o + 637], Brow[:, l, 0:1])
            S_.copy(stio[0:4, o + 637:o + 638], Mrow[:, l, 0:1])
        S_.dma(st_out[:, :], stio[:, :])
    assert ws["pos"] == len(order), (ws["pos"], len(order))
    S_.finish()
    S_.emit()
    return nc, S_


def _pad(a):
    a = np.ascontiguousarray(a, dtype=np.float32).reshape(128, -1)
    o = np.zeros((128, PIECE), np.float32)
    o[:, :a.shape[1]] = a
    return o


def _k3(w):
    K, N = w.shape
    return w.reshape(K // 128, 128, N).transpose(1, 0, 2)


def _pieces(inp, l):
    P = []
    wkv = _k3(inp["w_xkv"][l])
    for k in range(4):
        P.append(_pad(wkv[:, :, k * 512:(k + 1) * 512]))
    win = _k3(inp["w_in"][l])
    for k in range(4):
        P.append(_pad(win[:, :, k * 512:(k + 1) * 512]))
    P.append(_pad(win[:, :, 2048:2056]))
    qkv = np.stack([inp["w_q_c"][l], inp["w_k_c"][l], inp["w_v_c"][l]], 0)
    P.append(_pad(qkv.transpose(2, 0, 1, 3)))
    for name in ("w_out", "w_xq", "w_xo"):
        w = _k3(inp[name][l])
        for k in range(2):
            P.append(_pad(w[:, :, k * 512:(k + 1) * 512]))
    wup = _k3(inp["w_up"][l])
    for p in range(11):
        g = wup[:, :, p * 256:(p + 1) * 256]
        v = wup[:, :, DFF + p * 256:DFF + (p + 1) * 256]
        P.append(_pad(np.concatenate([g, v], axis=2)))
    wd = _k3(inp["w_down"][l])
    for n in range(8):
        P.append(_pad(wd[:, :, n * 128:(n + 1) * 128]))
    assert len(P) == PPL
    return P


def _col(v):
    v = np.asarray(v, np.float32)
    return v.reshape(-1, 128).T


def host_layout(inp, NL):
    wflat = np.stack([p for l in range(NL) for p in _pieces(inp, l)], 0)
    colp = np.zeros((128, NL * NCOL + 8), np.float32)
    rowp = np.zeros((NL, NROW), np.float32)
    for l in range(NL):
        o = l * NCOL
        colp[:, o + C_NMIX:o + C_NMIX + 8] = _col(inp["norm_mix_g"][l])
        colp[:, o + C_NX:o + C_NX + 8] = _col(inp["norm_x_g"][l])
        colp[:, o + C_NFFN:o + C_NFFN + 8] = _col(inp["norm_ffn_g"][l])
        cbw = inp["conv_b_w"][l]
        for jt in range(31):
            colp[:, o + C_CBW + jt * 2:o + C_CBW + jt * 2 + 2] = _col(cbw[jt])
        colp[:, o + C_CBB:o + C_CBB + 2] = _col(inp["conv_b_b"][l])
        colp[:, o + C_GNG:o + C_GNG + 2] = _col(inp["gn_b_g"][l])
        colp[:, o + C_GNB:o + C_GNB + 2] = _col(inp["gn_b_b"][l])
        ccw = inp["conv_c_w"][l]
        for jt in range(4):
            colp[:, o + C_CCW + jt * 4:o + C_CCW + jt * 4 + 4] = _col(ccw[jt])
        colp[:, o + C_CCB:o + C_CCB + 4] = _col(inp["conv_c_b"][l])
        colp[:, o + C_SKP:o + C_SKP + 4] = _col(inp["skip_c"][l])
        cfw = inp["conv_f_w"][l]
        for jt in range(3):
            colp[:, o + C_CFW + jt * 22:o + C_CFW + (jt + 1) * 22] = _col(cfw[jt])
        colp[:, o + C_CFB:o + C_CFB + 22] = _col(inp["conv_f_b"][l])
        colp[0:4, o + C_BIG] = inp["b_igate"][l]
        colp[0:4, o + C_BFG] = inp["b_fgate"][l]
        rowp[l, R_LNG:R_LNG + 256] = inp["ln_a_g"][l]
        rowp[l, R_LNB:R_LNB + 256] = inp["ln_a_b"][l]
        rowp[l, R_BS:R_BS + 512] = np.asarray(inp["b_s"][l]).reshape(-1)
        rowp[l, R_HNG:R_HNG + 512] = inp["hn_c_g"][l]
        rowp[l, R_MEMG:R_MEMG + D] = inp["norm_mem_g"][l]
    colp[:, NL * NCOL:NL * NCOL + 8] = _col(inp["final_g"])
    wsT = np.ascontiguousarray(np.asarray(inp["w_s"], np.float32)[:NL].transpose(0, 3, 1, 2)).reshape(NL, 128, 512)
    cst = np.zeros((128, NCST), np.float32)
    ii = np.arange(128)
    cst[:, K_ID:K_ID + 128] = np.eye(128, dtype=np.float32)
    cst[:, K_MLE:K_MLE + 128] = (ii[:, None] <= ii[None, :]).astype(np.float32)
    cst[:, K_MBIG:K_MBIG + 128] = np.where(ii[:, None] <= ii[None, :], 0.0, 1e30).astype(np.float32)
    cst[:, K_BLK:K_BLK + 128] = ((ii[:, None] // 64) == (ii[None, :] // 64)).astype(np.float32) / 64.0
    for hh in range(4):
        cst[hh, K_SEL + hh * 128:K_SEL + (hh + 1) * 128] = 1.0
    cst[:, K_ONE:K_ONE + 128] = 1.0
    return dict(wflat=wflat, colp=colp, rowp=rowp, wsT=wsT, cst=cst)


_CACHE = {}


def run(inp, NB, S, NL, ncores):
    key = (NB, S, NL, False)
    if key not in _CACHE:
        _CACHE[key] = build(NB, S, NL)[0]
    nc = _CACHE[key]
    shared = host_layout(inp, NL)
    x = np.asarray(inp["x"], np.float32)
    mem = np.asarray(inp["mem"], np.float32)
    in_maps = []
    for c in range(ncores):
        m = dict(shared)
        m["x"] = np.ascontiguousarray(x[c * NB:(c + 1) * NB])
        m["mem"] = np.ascontiguousarray(mem[c * NB:(c + 1) * NB])
        in_maps.append(m)
    res = run_bass_kernel_spmd(nc, in_maps, core_ids=list(range(ncores)))
    return np.concatenate([np.asarray(r["out"]) for r in res.results], axis=0)


def run_chunked(inp, SCH, NL, ncores):
    key = (1, SCH, NL, True)
    if key not in _CACHE:
        _CACHE[key] = build(1, SCH, NL, stateio=True)[0]
    nc = _CACHE[key]
    shared = host_layout(inp, NL)
    x = np.asarray(inp["x"], np.float32)
    mem = np.asarray(inp["mem"], np.float32)
    B, S = x.shape[0], x.shape[1]
    out = np.zeros((B, S, D), np.float32)
    for g in range(0, B, ncores):
        nb = min(ncores, B - g)
        state = [np.zeros((128, NL * SL), np.float32) for _ in range(nb)]
        for ch in range(S // SCH):
            in_maps = []
            for c in range(nb):
                m = dict(shared)
                m["x"] = np.ascontiguousarray(x[g + c:g + c + 1, ch * SCH:(ch + 1) * SCH])
                m["mem"] = np.ascontiguousarray(mem[g + c:g + c + 1])
                m["st_in"] = state[c]
                in_maps.append(m)
            res = run_bass_kernel_spmd(nc, in_maps, core_ids=list(range(nb)))
            for c in range(nb):
                out[g + c, ch * SCH:(ch + 1) * SCH] = np.asarray(res.results[c]["out"])[0]
                state[c] = np.ascontiguousarray(np.asarray(res.results[c]["st_out"], np.float32))
    return out


def kernel(**inputs):
    inp = {k: np.asarray(v) for k, v in inputs.items()}
    return run(inp, 2, 4096, 2, 8).astype(np.float32)
```

```python
import numpy as np
import concourse.bass as bass
import concourse.mybir as mybir
from concourse.bass_utils import run_bass_kernel_spmd

F32 = mybir.dt.float32
BF16 = mybir.dt.bfloat16
ALU = mybir.AluOpType
AF = mybir.ActivationFunctionType
AX = mybir.AxisListType

D = 1024
NMEM = 256
DFF = 2816
T = 512
NJ = 4
NSLOT = 6
PIECE = 4096
PPL = 35
RMS_EPS = 1e-6
LN_EPS = 1e-5

C_NMIX, C_NX, C_NFFN = 0, 8, 16
C_CBW, C_CBB, C_GNG, C_GNB = 24, 86, 88, 90
C_CCW, C_CCB, C_SKP = 92, 108, 112
C_CFW, C_CFB = 116, 182
C_BIG, C_BFG = 204, 205
NCOL = 208
R_LNG, R_LNB, R_BS, R_HNG, R_MEMG = 0, 256, 512, 1024, 1536
NROW = 2560
K_ID, K_MLE, K_MBIG, K_BLK, K_SEL, K_ONE = 0, 128, 256, 384, 512, 1024
NCST = 1152


def _es(dt):
    return 2 if dt == BF16 else 4


class Sched:
    ENGS = ("pe", "act", "dve", "pool", "sp")

    def __init__(self, nc, ndsem=16):
        self.nc = nc
        self.q = {e: [] for e in self.ENGS}
        self.sem = {e: nc.alloc_semaphore("sem_" + e) for e in self.ENGS}
        self.cnt = {e: 0 for e in self.ENGS}
        self.dsem = [nc.alloc_semaphore("dsem%d" % i) for i in range(ndsem)]
        self.dn = 0
        self.seen = {e: {} for e in self.ENGS}
        self.rec = {}
        self.nwait = 0
        self.phase = ""
        self.labels = {e: [] for e in self.ENGS}

    def _iv(self, ap):
        dims = ap.ap
        off = int(ap.offset)
        es = _es(ap.dtype)
        sp = str(ap.space).upper()
        if "PSUM" in sp:
            return ap.tensor.name, 0, 2048
        if "DRAM" in sp:
            lo = off
            free = dims
        else:
            ps = dims[0][0]
            lo = off % ps if ps > 0 else off
            free = dims[1:]
        ext = 1
        for s, c in free:
            ext += (c - 1) * abs(s)
        return ap.tensor.name, lo * es, (lo + ext) * es

    def _deps(self, reads, writes):
        deps = {}
        for ap in reads:
            name, lo, hi = self._iv(ap)
            R = self.rec.get(name)
            if R:
                for (l, h, t) in R["w"]:
                    if l < hi and lo < h:
                        deps[t[0]] = max(deps.get(t[0], 0), t[1])
        for ap in writes:
            name, lo, hi = self._iv(ap)
            R = self.rec.get(name)
            if R:
                for (l, h, t) in R["w"]:
                    if l < hi and lo < h:
                        deps[t[0]] = max(deps.get(t[0], 0), t[1])
                for (e, l, h), t in R["r"].items():
                    if l < hi and lo < h:
                        deps[t[0]] = max(deps.get(t[0], 0), t[1])
        return deps

    def _commit(self, reads, writes, ticket):
        for ap in reads:
            name, lo, hi = self._iv(ap)
            R = self.rec.setdefault(name, {"w": [], "r": {}})
            R["r"][(ticket[0], lo, hi)] = ticket
        for ap in writes:
            name, lo, hi = self._iv(ap)
            R = self.rec.setdefault(name, {"w": [], "r": {}})
            R["w"] = [(l, h, t) for (l, h, t) in R["w"] if not (lo <= l and h <= hi)]
            R["r"] = {k: t for k, t in R["r"].items() if not (lo <= k[1] and k[2] <= hi)}
            R["w"].append((lo, hi, ticket))

    def op(self, eng, fn, reads, writes):
        deps = self._deps(reads, writes)
        waits = []
        for k, v in deps.items():
            if k == "pe" and eng == "pe":
                continue
            if self.seen[eng].get(k, 0) < v:
                self.seen[eng][k] = v
                waits.append((k, v))
        self.cnt[eng] += 1
        ticket = (eng, self.cnt[eng])
        self.q[eng].append((waits, fn, ticket))
        self.labels[eng].append((self.phase, tuple(waits)))
        self.nwait += len(waits)
        self._commit(reads, writes, ticket)

    def dma(self, out, in_, eng="sp"):
        k = self.dn % len(self.dsem)
        use = self.dn // len(self.dsem)
        self.dn += 1
        key = ("d", k)
        deps = self._deps([in_], [out])
        if use > 0:
            deps[key] = max(deps.get(key, 0), 16 * use)
        waits = []
        for kk, v in deps.items():
            if self.seen[eng].get(kk, 0) < v:
                self.seen[eng][kk] = v
                waits.append((kk, v))
        ticket = (key, 16 * (use + 1))
        self.q[eng].append((waits, lambda e: e.dma_start(out=out, in_=in_), ticket))
        self._commit([in_], [out], ticket)

    def _semof(self, k):
        return self.dsem[k[1]] if isinstance(k, tuple) else self.sem[k]

    def finish(self):
        waits = []
        n = len(self.dsem)
        for k in range(n):
            uses = (self.dn - k + n - 1) // n if self.dn > k else 0
            if uses > 0:
                waits.append((("d", k), 16 * uses))
        self.q["sp"].append((waits, None, None))

    def emit(self):
        nc = self.nc
        S = self

        def replay(name):
            def f(eng):
                for waits, fn, ticket in S.q[name]:
                    for k, v in waits:
                        eng.wait_ge(S._semof(k), v)
                    if fn is None:
                        continue
                    ins = fn(eng)
                    if isinstance(ticket[0], tuple):
                        ins.then_inc(S._semof(ticket[0]), 16)
                    else:
                        ins.then_inc(S.sem[ticket[0]], 1)
            return f

        with nc.Block() as block:
            block.tensor(replay("pe"))
            block.scalar(replay("act"))
            block.vector(replay("dve"))
            block.gpsimd(replay("pool"))
            block.sync(replay("sp"))

    def mm(self, out, lhsT, rhs, start=True, stop=True):
        self.op("pe", lambda e: e.matmul(out, lhsT, rhs, start=start, stop=stop), [lhsT, rhs], [out])

    def tr(self, out, in_, ident):
        self.op("pe", lambda e: e.transpose(out, in_, ident), [in_, ident], [out])

    def act(self, out, in_, func, bias=None, scale=None, accum_out=None):
        reads = [in_]
        writes = [out]
        kw = {}
        if bias is not None:
            kw["bias"] = bias
            if not isinstance(bias, (int, float)):
                reads.append(bias)
        if scale is not None:
            kw["scale"] = scale
            if not isinstance(scale, (int, float)):
                reads.append(scale)
        if accum_out is not None:
            kw["accum_out"] = accum_out
            writes.append(accum_out)
        self.op("act", lambda e: e.activation(out, in_, func, **kw), reads, writes)

    def tt(self, out, in0, in1, op, eng="dve"):
        self.op(eng, lambda e: e.tensor_tensor(out, in0, in1, op), [in0, in1], [out])

    def ts(self, out, in0, s1, s2, op0, op1=None, eng="dve"):
        reads = [in0]
        for s in (s1, s2):
            if s is not None and not isinstance(s, (int, float)):
                reads.append(s)
        if op1 is None:
            self.op(eng, lambda e: e.tensor_scalar(out, in0, s1, None, op0), reads, [out])
        else:
            self.op(eng, lambda e: e.tensor_scalar(out, in0, s1, s2, op0, op1), reads, [out])

    def stt(self, out, in0, scalar, in1, op0, op1):
        reads = [in0, in1]
        if not isinstance(scalar, (int, float)):
            reads.append(scalar)
        self.op("dve", lambda e: e.scalar_tensor_tensor(out, in0, scalar, in1, op0, op1), reads, [out])

    def copy(self, out, in_, eng="dve"):
        if eng == "act":
            self.act(out, in_, AF.Copy)
        else:
            self.op(eng, lambda e: e.tensor_copy(out, in_), [in_], [out])

    def memset(self, out, val, eng="pool"):
        self.op(eng, lambda e: e.memset(out, val), [], [out])


SL = 638


def build(NB, S, NL, stateio=False):
    nc = bass.Bass("TRN2", target_bir_lowering=False)
    NT = S // T
    NP = NL * PPL
    x = nc.dram_tensor("x", [NB, S, D], F32, kind="ExternalInput")
    mem = nc.dram_tensor("mem", [NB, NMEM, D], F32, kind="ExternalInput")
    wflat = nc.dram_tensor("wflat", [NP, 128, PIECE], F32, kind="ExternalInput")
    colp = nc.dram_tensor("colp", [128, NL * NCOL + 8], F32, kind="ExternalInput")
    rowp = nc.dram_tensor("rowp", [NL, NROW], F32, kind="ExternalInput")
    wsT = nc.dram_tensor("wsT", [NL, 128, 512], F32, kind="ExternalInput")
    cst = nc.dram_tensor("cst", [128, NCST], F32, kind="ExternalInput")
    out = nc.dram_tensor("out", [NB, S, D], F32, kind="ExternalOutput")
    wbf = nc.dram_tensor("wbf", [NP, 128, PIECE], BF16)
    if stateio:
        st_in = nc.dram_tensor("st_in", [128, NL * SL], F32, kind="ExternalInput")
        st_out = nc.dram_tensor("st_out", [128, NL * SL], F32, kind="ExternalOutput")

    A = nc.alloc_sbuf_tensor
    cst_t = A("cst_t", [128, NCST], F32)
    identb = A("identb", [128, 128], BF16)
    onesb = A("onesb", [128, 128], BF16)
    blkb = A("blkb", [128, 128], BF16)
    neghalf = A("neghalf", [128, 512], F32)
    epsr = A("epsr", [128, 2], F32)
    colp_t = A("colp_t", [128, NL * NCOL + 8], F32)
    nbfg = A("nbfg", [4, NL], F32)
    lnab = A("lnab", [128, NL, 512], F32)
    bsb = A("bsb", [128, NL, 2, 128], F32)
    hngb = A("hngb", [128, NL, 512], F32)
    wsTb = A("wsTb", [128, NL, 512], BF16)
    h = A("h", [128, 8, T], F32)
    hn = A("hn", [128, 8, T], BF16)
    ring = A("ring", [128, NSLOT, PIECE], BF16)
    kTx = A("kTx", [128, NL, 8, NMEM], BF16)
    Vx = A("Vx", [128, NL, 2, D], BF16)
    Cst = A("Cst", [128, NL, 4, 128], F32)
    Cbf = A("Cbf", [128, NL, 4, 128], BF16)
    nst = A("nst", [128, NL, 4], F32)
    nbf = A("nbf", [128, NL, 4], BF16)
    yhist = A("yhist", [128, NL, 2, 32 + T], BF16)
    xcb = A("xcb", [128, NL, 4, 4 + T], BF16)
    fhist = A("fhist", [128, NL, 22, 2], F32)
    Brow = A("Brow", [4, NL, 129], F32)
    Mrow = A("Mrow", [4, NL, 129], F32)
    xin = A("xin", [128, 2, D], F32)
    ostage = A("ostage", [128, 2, D], F32)
    rstd = A("rstd", [128, T], F32)
    t0 = A("t0", [128, T], F32)
    sqn = A("sqn", [128, 2, T], BF16)
    small = A("small", [128, 64], F32)
    stio = A("stio", [128, NL * SL], F32)
    NA = 12960
    arena = A("arena", [128, NA], F32)
    psb = [nc.alloc_psum_tensor("ps%d" % i, [128, 512], F32) for i in range(8)]

    S_ = Sched(nc)
    st = {"ps": 0}

    def psum():
        p = psb[st["ps"] % 8]
        st["ps"] += 1
        return p

    class Carver:
        def __init__(self):
            self.o = 0

        def f32(self, *shape):
            n = int(np.prod(shape))
            v = arena[:, self.o:self.o + n]
            self.o += n
            assert self.o <= NA, self.o
            return self._shape(v, shape)

        def bf(self, *shape):
            n = int(np.prod(shape))
            n32 = (n + 1) // 2
            v = arena[:, self.o:self.o + n32].bitcast(BF16)[:, 0:n]
            self.o += n32
            assert self.o <= NA, self.o
            return self._shape(v, shape)

        @staticmethod
        def _shape(v, shape):
            if len(shape) == 1:
                return v
            if len(shape) == 2:
                return v.rearrange("p (a b) -> p a b", b=shape[1])
            return v.rearrange("p (a b c) -> p a b c", b=shape[1], c=shape[2])

    ident = cst_t[:, K_ID:K_ID + 128]
    mle = cst_t[:, K_MLE:K_MLE + 128]
    mbig = cst_t[:, K_MBIG:K_MBIG + 128]
    onesf = cst_t[:, K_ONE:K_ONE + 128]

    def colL(l, c, n=1, p0=0, p1=128):
        return colp_t[p0:p1, l * NCOL + c:l * NCOL + c + n]

    S_.dma(cst_t[:, :], cst[:, :])
    S_.dma(colp_t[:, :], colp[:, :])
    S_.copy(identb[:, :], ident)
    S_.memset(onesb[:, :], 1.0)
    S_.copy(blkb[:, :], cst_t[:, K_BLK:K_BLK + 128])
    S_.memset(neghalf[:, :], -0.5)
    S_.memset(epsr[:, 0:1], RMS_EPS)
    S_.memset(epsr[:, 1:2], LN_EPS)
    cv = Carver()
    wtmp = cv.f32(512)
    for l in range(NL):
        S_.dma(lnab[:, l, :], rowp[l, R_LNG:R_LNG + 512].partition_broadcast(128))
        S_.dma(hngb[:, l, :], rowp[l, R_HNG:R_HNG + 512].partition_broadcast(128))
        for ncn in range(2):
            for hf in range(2):
                hh = 2 * ncn + hf
                S_.dma(bsb[hf * 64:(hf + 1) * 64, l, ncn, :],
                       rowp[l, R_BS + hh * 128:R_BS + (hh + 1) * 128].partition_broadcast(64))
        S_.dma(wtmp, wsT[l, :, :])
        S_.tt(wsTb[:, l, :].rearrange("p (a b) -> p a b", b=128),
              wtmp.rearrange("p (a b) -> p a b", b=128),
              mle.unsqueeze(1).to_broadcast([128, 4, 128]), ALU.mult)
        S_.ts(nbfg[:, l:l + 1], colL(l, C_BFG, 1, 0, 4), -1.0, None, ALU.mult)

    cv = Carver()
    stage = [cv.f32(PIECE) for _ in range(3)]
    hb = h[:, :, :].rearrange("p a b -> p (a b)").bitcast(BF16)
    obf = [hb[:, 0:PIECE], hb[:, PIECE:2 * PIECE], hn[:, :, :].rearrange("p a b -> p (a b)")]
    cast_eng = ["dve", "act", "dve"]
    for i in range(NP):
        S_.dma(stage[i % 3], wflat[i, :, :])
        S_.copy(obf[i % 3], stage[i % 3], eng=cast_eng[i % 3])
        S_.dma(wbf[i, :, :], obf[i % 3])

    order = []
    for b in range(NB):
        for l in range(NL):
            order += [l * PPL + k for k in range(4)]
        for ti in range(NT):
            for l in range(NL):
                order += [l * PPL + k for k in range(4, PPL)]
    ws = {"issued": 0, "pos": 0}

    def wnext(expect):
        while ws["issued"] < min(len(order), ws["pos"] + NSLOT):
            i = ws["issued"]
            S_.dma(ring[:, i % NSLOT, :], wbf[order[i], :, :])
            ws["issued"] += 1
        assert order[ws["pos"]] == expect, (order[ws["pos"]], expect)
        slot = ws["pos"] % NSLOT
        ws["pos"] += 1
        return ring[:, slot, :]

    def w3(v, n):
        return v.rearrange("p (a b) -> p a b", b=n)

    def rms_stats(src):
        ps = psum()
        for c in range(8):
            S_.act(sqn[:, c % 2, :], src[:, c, :], AF.Square)
            S_.mm(ps[:, :], onesb[:, :], sqn[:, c % 2, :], start=(c == 0), stop=(c == 7))
        S_.act(t0[:, :], ps[:, :], AF.Ln, bias=epsr[:, 0:1], scale=1.0 / D)
        S_.act(rstd[:, :], t0[:, :], AF.Exp, scale=-0.5)

    def norm_to_hn(gcol):
        rms_stats(h)
        for c in range(8):
            S_.stt(hn[:, c, :], h[:, c, :], gcol[:, c:c + 1], rstd[:, :], ALU.mult, ALU.mult)

    def proj_fm(W, col0, rhs_t, nk, ps_ap):
        for kc in range(nk):
            S_.mm(ps_ap, W[:, kc, col0:col0 + 128], rhs_t[:, kc, :], start=(kc == 0), stop=(kc == nk - 1))

    def resid_proj(pid0, src):
        for half in range(2):
            W = w3(wnext(pid0 + half), 512)
            for n4 in range(4):
                ncn = half * 4 + n4
                ps = psum()
                proj_fm(W, n4 * 128, src, 8, ps[:, :])
                S_.tt(h[:, ncn, :], h[:, ncn, :], ps[:, :], ALU.add)

    def rsqrt_small(out, in_, eps, n):
        S_.ts(out, in_, eps, None, ALU.add)
        S_.tt(out, out, neghalf[0:out.shape[0], 0:n], ALU.pow, eng="pool")

    for b in range(NB):
        for l in range(NL):
            cv = Carver()
            memt = cv.f32(2, D)
            memg = cv.f32(D)
            mnb = cv.bf(2, D)
            mnT = cv.bf(8, NMEM)
            junk = cv.f32(D)
            S_.dma(memg, rowp[l, R_MEMG:R_MEMG + D].partition_broadcast(128))
            ss = small[:, 0:2]
            rs2 = small[:, 2:4]
            for mc in range(2):
                S_.dma(memt[:, mc, :], mem[b, mc * 128:(mc + 1) * 128, :])
                S_.act(junk, memt[:, mc, :], AF.Square, accum_out=ss[:, mc:mc + 1])
            S_.ts(rs2, ss, 1.0 / D, RMS_EPS, ALU.mult, ALU.add)
            S_.tt(rs2, rs2, neghalf[:, 0:2], ALU.pow, eng="pool")
            for mc in range(2):
                S_.stt(mnb[:, mc, :], memt[:, mc, :], rs2[:, mc:mc + 1], memg, ALU.mult, ALU.mult)
                for half in range(2):
                    ps = psum()
                    pv = ps[:, :].bitcast(BF16)
                    for k4 in range(4):
                        kc = half * 4 + k4
                        S_.tr(pv[:, k4 * 128:(k4 + 1) * 128], mnb[:, mc, kc * 128:(kc + 1) * 128], identb[:, :])
                    S_.copy(mnT[:, half * 4:half * 4 + 4, mc * 128:(mc + 1) * 128],
                            pv[:, 0:512].rearrange("p (a b) -> p a b", b=128), eng="act")
            for pk in range(2):
                W = w3(wnext(l * PPL + pk), 512)
                for d4 in range(4):
                    dch = pk * 4 + d4
                    ps = psum()
                    proj_fm(W, d4 * 128, mnT, 8, ps[:, 0:NMEM])
                    S_.copy(kTx[:, l, dch, :], ps[:, 0:NMEM], eng="act")
            for pv_ in range(2):
                W = w3(wnext(l * PPL + 2 + pv_), 512)
                for mc in range(2):
                    ps = psum()
                    for kc in range(8):
                        S_.mm(ps[:, :], mnT[:, kc, mc * 128:(mc + 1) * 128], W[:, kc, :], start=(kc == 0), stop=(kc == 7))
                    S_.copy(Vx[:, l, mc, pv_ * 512:(pv_ + 1) * 512], ps[:, :], eng="act")
        S_.memset(Cst[:, :, :, :], 0.0)
        S_.memset(Cbf[:, :, :, :], 0.0)
        S_.memset(nst[:, :, :], 0.0)
        S_.memset(nbf[:, :, :], 0.0)
        S_.memset(yhist[:, :, :, 0:32], 0.0)
        S_.memset(xcb[:, :, :, 0:4], 0.0)
        S_.memset(fhist[:, :, :, :], 0.0)
        S_.memset(Brow[:, :, 0:1], 0.0)
        S_.memset(Mrow[:, :, 0:1], 0.0)
        if stateio:
            S_.dma(stio[:, :], st_in[:, :])
            for l in range(NL):
                o = l * SL
                S_.copy(Cst[:, l, :, :], stio[:, o:o + 512].rearrange("p (a b) -> p a b", b=128))
                S_.copy(nst[:, l, :], stio[:, o + 512:o + 516])
                S_.copy(yhist[:, l, :, 2:32], stio[:, o + 516:o + 576].rearrange("p (a b) -> p a b", b=30))
                S_.copy(xcb[:, l, :, 0:4], stio[:, o + 576:o + 592].rearrange("p (a b) -> p a b", b=4))
                S_.copy(fhist[:, l, :, :], stio[:, o + 592:o + 636].rearrange("p (a b) -> p a b", b=2))
                S_.copy(Brow[:, l, 0:1], stio[0:4, o + 636:o + 637])
                S_.copy(Mrow[:, l, 0:1], stio[0:4, o + 637:o + 638])
                S_.copy(Cbf[:, l, :, :], Cst[:, l, :, :], eng="pool")
                S_.copy(nbf[:, l, :], nst[:, l, :], eng="pool")

        for ti in range(NT):
            tok0 = ti * T
            S_.phase = "xload"
            for j in range(NJ):
                S_.dma(xin[:, j % 2, :], x[b, tok0 + j * 128:tok0 + (j + 1) * 128, :])
                for half in range(2):
                    ps = psum()
                    for c4 in range(4):
                        c = half * 4 + c4
                        S_.tr(ps[:, c4 * 128:(c4 + 1) * 128], xin[:, j % 2, c * 128:(c + 1) * 128], ident)
                    S_.copy(h[:, half * 4:half * 4 + 4, j * 128:(j + 1) * 128],
                            ps[:, :].rearrange("p (a b) -> p a b", b=128), eng="act")

            for l in range(NL):
                pid = l * PPL
                S_.phase = "mixnorm"
                norm_to_hn(colL(l, C_NMIX, 8))
                cv = Carver()
                ycat = cv.bf(8, T)
                xconv = cv.bf(4, T)
                sgo = cv.bf(4, T)
                logi_row = cv.f32(T)
                sp_row = cv.f32(T)
                mark = cv.o
                S_.phase = "A"
                u = cv.bf(2, T)
                vn = cv.bf(4, 256)
                vg = [cv.f32(256) for _ in range(NJ)]
                W = w3(wnext(pid + 4), 512)
                for ncn in range(2):
                    ps = psum()
                    proj_fm(W, ncn * 128, hn, 8, ps[:, :])
                    S_.act(u[:, ncn, :], ps[:, :], AF.Gelu)
                for j in range(NJ):
                    js = slice(j * 128, (j + 1) * 128)
                    ps = psum()
                    for kc in range(8):
                        S_.mm(ps[:, 0:256], hn[:, kc, js], W[:, kc, 256:512], start=(kc == 0), stop=(kc == 7))
                    S_.act(vg[j], ps[:, 0:256], AF.Gelu)
                for j in range(NJ):
                    v_ = vg[j]
                    so = 8 + (j % 2) * 12
                    st6 = small[:, so:so + 6]
                    mv = small[:, so + 6:so + 8]
                    rsa = small[:, so + 8:so + 9]
                    S_.op("dve", lambda e, o=st6, i=v_: e.bn_stats(o, i), [v_], [st6])
                    S_.op("dve", lambda e, o=mv, i=st6: e.bn_aggr(o, i), [st6], [mv])
                    rsqrt_small(rsa, mv[:, 1:2], LN_EPS, 1)
                    S_.ts(v_, v_, mv[:, 0:1], rsa, ALU.subtract, ALU.mult)
                    S_.tt(v_, v_, lnab[:, l, 0:256], ALU.mult)
                    S_.tt(vn[:, j, :], v_, lnab[:, l, 256:512], ALU.add)
                cv.o = mark
                colsJ = [cv.f32(20) for _ in range(NJ)]
                wTJ = [cv.f32(4, 128) for _ in range(NJ)]
                r2 = cv.o
                a_R = cv.f32(4, 128)
                rtmp = cv.f32(128)
                arg = cv.f32(4, 128)
                r3 = cv.o
                cv.o = r2
                sgB = cv.f32(T)
                tmpa = cv.f32(2, 128)
                cv.o = r3
                y2 = cv.f32(T)
                y2b = cv.bf(T)
                dB = cv.f32(T)
                sqd = cv.bf(T)
                t1 = cv.f32(T)
                tmpB = cv.f32(T)
                diag = cv.bf(31, 128)
                cv.o = r3
                cvt = cv.f32(T)
                sgc = cv.f32(T)
                cv.o = r3
                qTb = cv.bf(4, 128)
                kTb = cv.bf(4, 128)
                kwb = cv.bf(4, 128)
                vtb = cv.bf(4, 128)
                scT = cv.bf(4, 128)
                n1 = cv.f32(4, 128)
                hs = cv.f32(4, 128)
                hnt = cv.f32(4, 128)
                t2 = n1
                S_.phase = "B"
                W = w3(wnext(pid + 5), 512)
                for c in range(2):
                    psa = psum()
                    proj_fm(W, c * 128, hn, 8, psa[:, :])
                    psg = psum()
                    proj_fm(W, 256 + c * 128, hn, 8, psg[:, :])
                    S_.act(sgB, psg[:, :], AF.Sigmoid)
                    S_.tt(yhist[:, l, c, 32:32 + T], psa[:, :], sgB, ALU.mult)
                S_.phase = "Cproj"
                W = w3(wnext(pid + 6), 512)
                for hh in range(4):
                    ps = psum()
                    proj_fm(W, hh * 128, hn, 8, ps[:, :])
                    S_.copy(xcb[:, l, hh, 4:4 + T], ps[:, :], eng="act")
                W = w3(wnext(pid + 7), 512)
                for hh in range(4):
                    ps = psum()
                    proj_fm(W, hh * 128, hn, 8, ps[:, :])
                    S_.act(sgo[:, hh, :], ps[:, :], AF.Sigmoid)
                W = w3(wnext(pid + 8)[:, 0:64], 8)
                psi = psum()
                for kc in range(8):
                    S_.mm(psi[0:4, :], W[:, kc, 0:4], hn[:, kc, :], start=(kc == 0), stop=(kc == 7))
                psf = psum()
                for kc in range(8):
                    S_.mm(psf[0:4, :], W[:, kc, 4:8], hn[:, kc, :], start=(kc == 0), stop=(kc == 7))
                for hh in range(4):
                    S_.ts(cvt, xcb[:, l, hh, 1:1 + T], colL(l, C_CCW + hh), colL(l, C_CCB + hh), ALU.mult, ALU.add)
                    for jt in range(1, 4):
                        S_.stt(cvt, xcb[:, l, hh, 1 + jt:1 + jt + T], colL(l, C_CCW + jt * 4 + hh), cvt, ALU.mult, ALU.add)
                    S_.act(sgc, cvt, AF.Sigmoid)
                    S_.tt(xconv[:, hh, :], cvt, sgc, ALU.mult)
                S_.phase = "A"
                for j in range(NJ):
                    js = slice(j * 128, (j + 1) * 128)
                    ps = psum()
                    for hh in range(4):
                        p0 = (hh % 2) * 64
                        S_.mm(ps[p0:p0 + 64, (hh // 2) * 128:(hh // 2 + 1) * 128],
                              vn[:, j, hh * 64:(hh + 1) * 64], wsTb[:, l, hh * 128:(hh + 1) * 128])
                    S_.tt(tmpa, ps[:, 0:256].rearrange("p (a b) -> p a b", b=128), bsb[:, l, :, :], ALU.add)
                    S_.tt(ycat[:, 0:2, js], tmpa, u[:, 0:2, js], ALU.mult)
                S_.phase = "Cgate"
                S_.act(logi_row[0:4, :], psi[0:4, :], AF.Identity, bias=colL(l, C_BIG, 1, 0, 4))
                S_.act(sp_row[0:4, :], psf[0:4, :], AF.Exp, bias=nbfg[:, l:l + 1], scale=-1.0)
                S_.act(sp_row[0:4, :], sp_row[0:4, :], AF.Ln, bias=1.0)
                S_.phase = "B"
                for c in range(2):
                    for jt in range(31):
                        S_.act(diag[:, jt, :], identb[:, :], AF.Copy, scale=colL(l, C_CBW + jt * 2 + c))
                    psc31 = psum()
                    for jt in range(31):
                        S_.mm(psc31[:, :], diag[:, jt, :], yhist[:, l, c, 2 + jt:2 + jt + T], start=(jt == 0), stop=(jt == 30))
                    S_.copy(yhist[:, l, c, 0:32], yhist[:, l, c, T:T + 32], eng="pool")
                    S_.act(y2, psc31[:, :], AF.Identity, bias=colL(l, C_CBB + c))
                    S_.act(y2b, psc31[:, :], AF.Identity, bias=colL(l, C_CBB + c))
                    if c == 0:
                        S_.phase = "Cgate"
                        for j in range(NJ):
                            js = slice(j * 128, (j + 1) * 128)
                            cols = colsJ[j]
                            wT = wTJ[j]
                            Bn = Brow[:, l, 1:129]
                            Mn = Mrow[:, l, 1:129]
                            S_.op("dve", lambda e, o=Bn, d1=sp_row[0:4, js], i0=Brow[:, l, 0:1]:
                                  e.tensor_tensor_scan(o, onesf[0:4, :], d1, i0, ALU.mult, ALU.subtract),
                                  [onesf[0:4, :], sp_row[0:4, js], Brow[:, l, 0:1]], [Bn])
                            S_.tt(a_R[0:4, 0, :], logi_row[0:4, js], Bn, ALU.subtract)
                            S_.op("dve", lambda e, o=Mn, d1=a_R[0:4, 0, :], i0=Mrow[:, l, 0:1]:
                                  e.tensor_tensor_scan(o, onesf[0:4, :], d1, i0, ALU.mult, ALU.max),
                                  [onesf[0:4, :], a_R[0:4, 0, :], Mrow[:, l, 0:1]], [Mn])
                            S_.act(a_R[0:4, 1, :], Mn, AF.Exp, bias=Mrow[:, l, 0:1], scale=-1.0)
                            S_.tt(rtmp[0:4, :], Bn, Mn, ALU.add)
                            S_.act(a_R[0:4, 2, :], rtmp[0:4, :], AF.Exp, scale=-1.0)
                            negM = small[0:4, 20:21]
                            S_.ts(negM, Mrow[:, l, 128:129], -1.0, None, ALU.mult)
                            S_.act(a_R[0:4, 3, :], a_R[0:4, 0, :], AF.Exp, bias=negM)
                            dg = small[0:4, 24:28]
                            S_.ts(dg, ident[0:4, 0:4], a_R[0:4, 1, 127:128], None, ALU.mult)
                            psc = psum()
                            for q in range(4):
                                S_.tr(psc[:, q * 4:(q + 1) * 4], a_R[0:4, q, :], ident[0:4, 0:4])
                            S_.mm(psc[:, 16:20], onesf[0:4, :], dg)
                            S_.copy(cols, psc[:, 0:20])
                            psM = psum()
                            for hh in range(4):
                                S_.mm(psM[:, hh * 128:(hh + 1) * 128], cst_t[0:4, K_SEL + hh * 128:K_SEL + (hh + 1) * 128], Mn)
                            S_.tt(arg, psM[:, :].rearrange("p (a b) -> p a b", b=128),
                                  mbig.unsqueeze(1).to_broadcast([128, 4, 128]), ALU.add)
                            S_.copy(Brow[:, l, 0:1], Brow[:, l, 128:129], eng="pool")
                            S_.copy(Mrow[:, l, 0:1], Mrow[:, l, 128:129], eng="pool")
                            for hh in range(4):
                                S_.act(wT[:, hh, :], arg[:, hh, :], AF.Exp, bias=cols[:, hh:hh + 1], scale=-1.0)
                        S_.phase = "B"
                    psm = psum()
                    S_.mm(psm[:, :], blkb[:, :], y2b)
                    S_.tt(dB, y2, psm[:, :], ALU.subtract)
                    S_.act(sqd, dB, AF.Square)
                    psv = psum()
                    S_.mm(psv[:, :], blkb[:, :], sqd)
                    S_.act(t1, psv[:, :], AF.Ln, bias=epsr[:, 1:2])
                    S_.act(t1, t1, AF.Exp, scale=-0.5)
                    S_.stt(tmpB, dB, colL(l, C_GNG + c), t1, ALU.mult, ALU.mult)
                    S_.act(y2, tmpB, AF.Sigmoid, bias=colL(l, C_GNB + c))
                    S_.stt(ycat[:, 2 + c, :], tmpB, colL(l, C_GNB + c), y2, ALU.add, ALU.mult)
                Wq = wnext(pid + 9)[:, 0:1536].rearrange("p (a b c) -> p a b c", b=4, c=128)
                S_.phase = "Cloop"
                for j in range(NJ):
                    js = slice(j * 128, (j + 1) * 128)
                    psq = psum()
                    for hh in range(4):
                        S_.mm(psq[:, hh * 128:(hh + 1) * 128], Wq[:, 0, hh, :], xconv[:, hh, js])
                    S_.act(qTb, psq[:, :].rearrange("p (a b) -> p a b", b=128), AF.Copy, scale=128.0 ** -0.5)
                    psk = psum()
                    for hh in range(4):
                        S_.mm(psk[:, hh * 128:(hh + 1) * 128], Wq[:, 1, hh, :], xconv[:, hh, js])
                    S_.copy(kTb, psk[:, :].rearrange("p (a b) -> p a b", b=128), eng="act")
                    pskt = psum()
                    for hh in range(4):
                        S_.mm(pskt[:, hh * 128:(hh + 1) * 128], xconv[:, hh, js], Wq[:, 1, hh, :])
                    psvt = psum()
                    for hh in range(4):
                        S_.mm(psvt[:, hh * 128:(hh + 1) * 128], xcb[:, l, hh, 4 + j * 128:4 + (j + 1) * 128], Wq[:, 2, hh, :])
                    S_.copy(vtb, psvt[:, :].rearrange("p (a b) -> p a b", b=128), eng="act")
                    pss = psum()
                    for hh in range(4):
                        S_.mm(pss[:, hh * 128:(hh + 1) * 128], kTb[:, hh, :], qTb[:, hh, :])
                    psP1 = psum()
                    psD = psum()
                    for hh in range(4):
                        S_.mm(psP1[:, hh * 128:(hh + 1) * 128], qTb[:, hh, :], Cbf[:, l, hh, :])
                        S_.mm(psD[:, hh:hh + 1], qTb[:, hh, :], nbf[:, l, hh:hh + 1])
                    cols = colsJ[j]
                    wT = wTJ[j]
                    S_.tt(kwb, pskt[:, :].rearrange("p (a b) -> p a b", b=128),
                          cols[:, 12:16].unsqueeze(2).to_broadcast([128, 4, 128]), ALU.mult)
                    S_.tt(scT, pss[:, :].rearrange("p (a b) -> p a b", b=128), wT, ALU.mult)
                    psP2 = psum()
                    psD_b = psum()
                    for hh in range(4):
                        S_.mm(psP2[:, hh * 128:(hh + 1) * 128], scT[:, hh, :], vtb[:, hh, :])
                        S_.mm(psD_b[:, hh:hh + 1], scT[:, hh, :], onesb[:, 0:1])
                    den = small[:, 28:32]
                    rden = small[:, 32:36]
                    S_.tt(den, psD[:, 0:4], cols[:, 4:8], ALU.mult)
                    S_.tt(den, den, psD_b[:, 0:4], ALU.add)
                    S_.stt(den, den, -1.0, den, ALU.mult, ALU.max)
                    S_.tt(den, den, cols[:, 8:12], ALU.max)
                    S_.op("dve", lambda e, o=rden, i=den: e.reciprocal(o, i), [den], [rden])
                    for hh in range(4):
                        S_.act(n1[:, hh, :], psP1[:, hh * 128:(hh + 1) * 128], AF.Copy, scale=cols[:, 4 + hh:5 + hh])
                    S_.tt(hs, n1, psP2[:, :].rearrange("p (a b) -> p a b", b=128), ALU.add)
                    S_.tt(hs, hs, rden.unsqueeze(2).to_broadcast([128, 4, 128]), ALU.mult)
                    st24 = small[:, 36:60].rearrange("p (a b) -> p a b", b=6)
                    mv8 = small[:, 0:8].rearrange("p (a b) -> p a b", b=2)
                    for hh in range(4):
                        S_.op("dve", lambda e, o=st24[:, hh, :], i=hs[:, hh, :]: e.bn_stats(o, i), [hs[:, hh, :]], [st24[:, hh, :]])
                        S_.op("dve", lambda e, o=mv8[:, hh, :], i=st24[:, hh, :]: e.bn_aggr(o, i), [st24[:, hh, :]], [mv8[:, hh, :]])
                    rsc = small[:, 60:64]
                    S_.ts(rsc, mv8[:, :, 1], LN_EPS, None, ALU.add)
                    S_.tt(rsc, rsc, neghalf[:, 0:4], ALU.pow, eng="pool")
                    for hh in range(4):
                        S_.ts(hnt[:, hh, :], hs[:, hh, :], mv8[:, hh, 0:1], rsc[:, hh:hh + 1], ALU.subtract, ALU.mult)
                    S_.tt(hnt, hnt, hngb[:, l, :].rearrange("p (a b) -> p a b", b=128), ALU.mult)
                    psT = psum()
                    for hh in range(4):
                        S_.tr(psT[:, hh * 128:(hh + 1) * 128], hnt[:, hh, :], ident)
                    for hh in range(4):
                        S_.stt(t2[:, hh, :], xconv[:, hh, js], colL(l, C_SKP + hh), psT[:, hh * 128:(hh + 1) * 128], ALU.mult, ALU.add)
                    S_.tt(ycat[:, 4:8, js], t2, sgo[:, :, js], ALU.mult)
                    psU = psum()
                    psD2 = psum()
                    for hh in range(4):
                        S_.mm(psU[:, hh * 128:(hh + 1) * 128], kwb[:, hh, :], vtb[:, hh, :])
                        S_.mm(psD2[:, hh:hh + 1], kwb[:, hh, :], onesb[:, 0:1])
                    for hh in range(4):
                        S_.stt(Cst[:, l, hh, :], Cst[:, l, hh, :], cols[:, 16 + hh:17 + hh], psU[:, hh * 128:(hh + 1) * 128], ALU.mult, ALU.add)
                    S_.tt(nst[:, l, :], nst[:, l, :], cols[:, 16:20], ALU.mult)
                    S_.tt(nst[:, l, :], nst[:, l, :], psD2[:, 0:4], ALU.add)
                    S_.copy(Cbf[:, l, :, :], Cst[:, l, :, :], eng="pool")
                    S_.copy(nbf[:, l, :], nst[:, l, :], eng="pool")
                for hh in range(4):
                    S_.copy(xcb[:, l, hh, 0:4], xcb[:, l, hh, T:T + 4], eng="pool")
                S_.phase = "outproj"
                resid_proj(pid + 10, ycat)

                S_.phase = "xattn"
                norm_to_hn(colL(l, C_NX, 8))
                cv = Carver()
                qx = cv.bf(8, T)
                oT = cv.bf(8, T)
                pT = cv.bf(8, T)
                pexp2 = [cv.bf(4, NMEM) for _ in range(2)]
                pn2 = [cv.bf(4, NMEM) for _ in range(2)]
                for half in range(2):
                    W = w3(wnext(pid + 12 + half), 512)
                    for n4 in range(4):
                        ps = psum()
                        proj_fm(W, n4 * 128, hn, 8, ps[:, :])
                        S_.copy(qx[:, half * 4 + n4, :], ps[:, :], eng="act")
                def qk_scores(j):
                    js = slice(j * 128, (j + 1) * 128)
                    pss2 = [psum(), psum()]
                    for hh in range(4):
                        pp = pss2[hh // 2]
                        o0 = (hh % 2) * NMEM
                        for dc in range(2):
                            S_.mm(pp[:, o0:o0 + NMEM], qx[:, hh * 2 + dc, js], kTx[:, l, hh * 2 + dc, :], start=(dc == 0), stop=(dc == 1))
                    return pss2

                pend = qk_scores(0)
                for j in range(NJ):
                    js = slice(j * 128, (j + 1) * 128)
                    so = (j % 2) * 16
                    mx = small[:, so + 0:so + 4]
                    nmx = small[:, so + 4:so + 8]
                    ssum = small[:, so + 8:so + 12]
                    rsx = small[:, so + 12:so + 16]
                    pexp = pexp2[j % 2]
                    pn = pn2[j % 2]
                    pss2 = pend
                    if j + 1 < NJ:
                        pend = qk_scores(j + 1)
                    for pr in range(2):
                        S_.op("dve", lambda e, o=mx[:, pr * 2:pr * 2 + 2], i=pss2[pr][:, :].rearrange("p (a b) -> p a b", b=NMEM):
                              e.tensor_reduce(o, i, AX.X, ALU.max),
                              [pss2[pr][:, :]], [mx[:, pr * 2:pr * 2 + 2]])
                    S_.ts(nmx, mx, -1.0 / 16.0, None, ALU.mult)
                    for hh in range(4):
                        pp = pss2[hh // 2]
                        o0 = (hh % 2) * NMEM
                        S_.act(pexp[:, hh, :], pp[:, o0:o0 + NMEM], AF.Exp, bias=nmx[:, hh:hh + 1], scale=1.0 / 16.0,
                               accum_out=ssum[:, hh:hh + 1])
                    S_.op("dve", lambda e, o=rsx, i=ssum: e.reciprocal(o, i), [ssum], [rsx])
                    S_.tt(pn, pexp, rsx.unsqueeze(2).to_broadcast([128, 4, NMEM]), ALU.mult)
                    ps = psum()
                    pv = ps[:, :].bitcast(BF16)
                    for hh in range(4):
                        for mc in range(2):
                            q_ = hh * 2 + mc
                            S_.tr(pv[:, q_ * 128:(q_ + 1) * 128], pn[:, hh, mc * 128:(mc + 1) * 128], identb[:, :])
                    S_.copy(pT[:, :, js], pv[:, :].rearrange("p (a b) -> p a b", b=128), eng="act")
                for hh in range(4):
                    for dc in range(2):
                        ps = psum()
                        for mc in range(2):
                            S_.mm(ps[:, :], Vx[:, l, mc, hh * 256 + dc * 128:hh * 256 + (dc + 1) * 128], pT[:, hh * 2 + mc, :],
                                  start=(mc == 0), stop=(mc == 1))
                        S_.copy(oT[:, hh * 2 + dc, :], ps[:, :], eng="act")
                resid_proj(pid + 14, oT)

                S_.phase = "ffn"
                norm_to_hn(colL(l, C_NFFN, 8))
                cv = Carver()
                actb = cv.bf(22, T)
                gx = [cv.f32(T + 2) for _ in range(2)]
                acc = [cv.f32(T) for _ in range(2)]
                gl = [cv.f32(T) for _ in range(2)]
                for p in range(11):
                    W = w3(wnext(pid + 16 + p), 512)
                    for cc in range(2):
                        c = 2 * p + cc
                        psg = psum()
                        proj_fm(W, cc * 128, hn, 8, psg[:, :])
                        psv = psum()
                        proj_fm(W, 256 + cc * 128, hn, 8, psv[:, :])
                        g_ = gx[c % 2]
                        a_ = acc[c % 2]
                        l_ = gl[c % 2]
                        S_.copy(g_[:, 0:2], fhist[:, l, c, :], eng="pool")
                        S_.copy(g_[:, 2:T + 2], psg[:, :], eng="act")
                        S_.copy(fhist[:, l, c, :], g_[:, T:T + 2], eng="pool")
                        S_.act(a_, psg[:, :], AF.Identity, bias=colL(l, C_CFB + c), scale=colL(l, C_CFW + 2 * 22 + c))
                        S_.stt(a_, g_[:, 1:T + 1], colL(l, C_CFW + 1 * 22 + c), a_, ALU.mult, ALU.add)
                        S_.stt(a_, g_[:, 0:T], colL(l, C_CFW + c), a_, ALU.mult, ALU.add)
                        S_.act(l_, a_, AF.Gelu)
                        S_.tt(actb[:, c, :], l_, psv[:, :], ALU.mult)
                for ncn in range(8):
                    W = w3(wnext(pid + 27 + ncn)[:, 0:22 * 128], 128)
                    ps = psum()
                    for kc in range(22):
                        S_.mm(ps[:, :], W[:, kc, :], actb[:, kc, :], start=(kc == 0), stop=(kc == 21))
                    S_.tt(h[:, ncn, :], h[:, ncn, :], ps[:, :], ALU.add)

            S_.phase = "final"
            rms_stats(h)
            cv = Carver()
            of = cv.f32(8, T)
            fg = colp_t[:, NL * NCOL:NL * NCOL + 8]
            for c in range(8):
                S_.stt(of[:, c, :], h[:, c, :], fg[:, c:c + 1], rstd[:, :], ALU.mult, ALU.mult)
            for j in range(NJ):
                js = slice(j * 128, (j + 1) * 128)
                for half in range(2):
                    ps = psum()
                    for c4 in range(4):
                        c = half * 4 + c4
                        S_.tr(ps[:, c4 * 128:(c4 + 1) * 128], of[:, c, js], ident)
                    S_.copy(ostage[:, j % 2, half * 512:(half + 1) * 512], ps[:, :], eng="act")
                S_.dma(out[b, tok0 + j * 128:tok0 + (j + 1) * 128, :], ostage[:, j % 2, :])

    if stateio:
        S_.memset(stio[:, :], 0.0)
        for l in range(NL):
            o = l * SL
            S_.copy(stio[:, o:o + 512].rearrange("p (a b) -> p a b", b=128), Cst[:, l, :, :])
            S_.copy(stio[:, o + 512:o + 516], nst[:, l, :])
            S_.copy(stio[:, o + 516:o + 576].rearrange("p (a b) -> p a b", b=30), yhist[:, l, :, 2:32])
            S_.copy(stio[:, o + 576:o + 592].rearrange("p (a b) -> p a b", b=4), xcb[:, l, :, 0:4])
            S_.copy(stio[:, o + 592:o + 636].rearrange("p (a b) -> p a b", b=2), fhist[:, l, :, :])
            S_.copy(stio[0:4, o + 636:o + 637], Brow[:, l, 0:1])
            S_.copy(stio[0:4, o + 637:o + 638], Mrow[:, l, 0:1])
        S_.dma(st_out[:, :], stio[:, :])
    assert ws["pos"] == len(order), (ws["pos"], len(order))
    S_.finish()
    S_.emit()
    return nc, S_


def _pad(a):
    a = np.ascontiguousarray(a, dtype=np.float32).reshape(128, -1)
    o = np.zeros((128, PIECE), np.float32)
    o[:, :a.shape[1]] = a
    return o


def _k3(w):
    K, N = w.shape
    return w.reshape(K // 128, 128, N).transpose(1, 0, 2)


def _pieces(inp, l):
    P = []
    wkv = _k3(inp["w_xkv"][l])
    for k in range(4):
        P.append(_pad(wkv[:, :, k * 512:(k + 1) * 512]))
    win = _k3(inp["w_in"][l])
    for k in range(4):
        P.append(_pad(win[:, :, k * 512:(k + 1) * 512]))
    P.append(_pad(win[:, :, 2048:2056]))
    qkv = np.stack([inp["w_q_c"][l], inp["w_k_c"][l], inp["w_v_c"][l]], 0)
    P.append(_pad(qkv.transpose(2, 0, 1, 3)))
    for name in ("w_out", "w_xq", "w_xo"):
        w = _k3(inp[name][l])
        for k in range(2):
            P.append(_pad(w[:, :, k * 512:(k + 1) * 512]))
    wup = _k3(inp["w_up"][l])
    for p in range(11):
        g = wup[:, :, p * 256:(p + 1) * 256]
        v = wup[:, :, DFF + p * 256:DFF + (p + 1) * 256]
        P.append(_pad(np.concatenate([g, v], axis=2)))
    wd = _k3(inp["w_down"][l])
    for n in range(8):
        P.append(_pad(wd[:, :, n * 128:(n + 1) * 128]))
    assert len(P) == PPL
    return P


def _col(v):
    v = np.asarray(v, np.float32)
    return v.reshape(-1, 128).T


def host_layout(inp, NL):
    wflat = np.stack([p for l in range(NL) for p in _pieces(inp, l)], 0)
    colp = np.zeros((128, NL * NCOL + 8), np.float32)
    rowp = np.zeros((NL, NROW), np.float32)
    for l in range(NL):
        o = l * NCOL
        colp[:, o + C_NMIX:o + C_NMIX + 8] = _col(inp["norm_mix_g"][l])
        colp[:, o + C_NX:o + C_NX + 8] = _col(inp["norm_x_g"][l])
        colp[:, o + C_NFFN:o + C_NFFN + 8] = _col(inp["norm_ffn_g"][l])
        cbw = inp["conv_b_w"][l]
        for jt in range(31):
            colp[:, o + C_CBW + jt * 2:o + C_CBW + jt * 2 + 2] = _col(cbw[jt])
        colp[:, o + C_CBB:o + C_CBB + 2] = _col(inp["conv_b_b"][l])
        colp[:, o + C_GNG:o + C_GNG + 2] = _col(inp["gn_b_g"][l])
        colp[:, o + C_GNB:o + C_GNB + 2] = _col(inp["gn_b_b"][l])
        ccw = inp["conv_c_w"][l]
        for jt in range(4):
            colp[:, o + C_CCW + jt * 4:o + C_CCW + jt * 4 + 4] = _col(ccw[jt])
        colp[:, o + C_CCB:o + C_CCB + 4] = _col(inp["conv_c_b"][l])
        colp[:, o + C_SKP:o + C_SKP + 4] = _col(inp["skip_c"][l])
        cfw = inp["conv_f_w"][l]
        for jt in range(3):
            colp[:, o + C_CFW + jt * 22:o + C_CFW + (jt + 1) * 22] = _col(cfw[jt])
        colp[:, o + C_CFB:o + C_CFB + 22] = _col(inp["conv_f_b"][l])
        colp[0:4, o + C_BIG] = inp["b_igate"][l]
        colp[0:4, o + C_BFG] = inp["b_fgate"][l]
        rowp[l, R_LNG:R_LNG + 256] = inp["ln_a_g"][l]
        rowp[l, R_LNB:R_LNB + 256] = inp["ln_a_b"][l]
        rowp[l, R_BS:R_BS + 512] = np.asarray(inp["b_s"][l]).reshape(-1)
        rowp[l, R_HNG:R_HNG + 512] = inp["hn_c_g"][l]
        rowp[l, R_MEMG:R_MEMG + D] = inp["norm_mem_g"][l]
    colp[:, NL * NCOL:NL * NCOL + 8] = _col(inp["final_g"])
    wsT = np.ascontiguousarray(np.asarray(inp["w_s"], np.float32)[:NL].transpose(0, 3, 1, 2)).reshape(NL, 128, 512)
    cst = np.zeros((128, NCST), np.float32)
    ii = np.arange(128)
    cst[:, K_ID:K_ID + 128] = np.eye(128, dtype=np.float32)
    cst[:, K_MLE:K_MLE + 128] = (ii[:, None] <= ii[None, :]).astype(np.float32)
    cst[:, K_MBIG:K_MBIG + 128] = np.where(ii[:, None] <= ii[None, :], 0.0, 1e30).astype(np.float32)
    cst[:, K_BLK:K_BLK + 128] = ((ii[:, None] // 64) == (ii[None, :] // 64)).astype(np.float32) / 64.0
    for hh in range(4):
        cst[hh, K_SEL + hh * 128:K_SEL + (hh + 1) * 128] = 1.0
    cst[:, K_ONE:K_ONE + 128] = 1.0
    return dict(wflat=wflat, colp=colp, rowp=rowp, wsT=wsT, cst=cst)


_CACHE = {}


def run(inp, NB, S, NL, ncores):
    key = (NB, S, NL, False)
    if key not in _CACHE:
        _CACHE[key] = build(NB, S, NL)[0]
    nc = _CACHE[key]
    shared = host_layout(inp, NL)
    x = np.asarray(inp["x"], np.float32)
    mem = np.asarray(inp["mem"], np.float32)
    in_maps = []
    for c in range(ncores):
        m = dict(shared)
        m["x"] = np.ascontiguousarray(x[c * NB:(c + 1) * NB])
        m["mem"] = np.ascontiguousarray(mem[c * NB:(c + 1) * NB])
        in_maps.append(m)
    res = run_bass_kernel_spmd(nc, in_maps, core_ids=list(range(ncores)))
    return np.concatenate([np.asarray(r["out"]) for r in res.results], axis=0)


def run_chunked(inp, SCH, NL, ncores):
    key = (1, SCH, NL, True)
    if key not in _CACHE:
        _CACHE[key] = build(1, SCH, NL, stateio=True)[0]
    nc = _CACHE[key]
    shared = host_layout(inp, NL)
    x = np.asarray(inp["x"], np.float32)
    mem = np.asarray(inp["mem"], np.float32)
    B, S = x.shape[0], x.shape[1]
    out = np.zeros((B, S, D), np.float32)
    for g in range(0, B, ncores):
        nb = min(ncores, B - g)
        state = [np.zeros((128, NL * SL), np.float32) for _ in range(nb)]
        for ch in range(S // SCH):
            in_maps = []
            for c in range(nb):
                m = dict(shared)
                m["x"] = np.ascontiguousarray(x[g + c:g + c + 1, ch * SCH:(ch + 1) * SCH])
                m["mem"] = np.ascontiguousarray(mem[g + c:g + c + 1])
                m["st_in"] = state[c]
                in_maps.append(m)
            res = run_bass_kernel_spmd(nc, in_maps, core_ids=list(range(nb)))
            for c in range(nb):
                out[g + c, ch * SCH:(ch + 1) * SCH] = np.asarray(res.results[c]["out"])[0]
                state[c] = np.ascontiguousarray(np.asarray(res.results[c]["st_out"], np.float32))
    return out


def kernel(**inputs):
    inp = {k: np.asarray(v) for k, v in inputs.items()}
    return run(inp, 2, 4096, 2, 8).astype(np.float32)
```
